# Optimizing a Trainium2 kernel written in Bass

```python
import math
import jax
import jax.numpy as jnp
from jax import lax
import numpy as np

D_MODEL = 1024
BATCH = 8
SEQ = 4096
DEPTH = 1

CHUNK = 64
EPS = 1e-6

GDN_HEADS = 8
GDN_DK = 128
GDN_DV = 128
GDN_QK_W = GDN_HEADS * GDN_DK
GDN_V_W = GDN_HEADS * GDN_DV
CONV_K = 4

HGRN_HEADS = 8
HGRN_DK = 128
HGRN_DV = 128
HGRN_K_W = HGRN_HEADS * HGRN_DK
HGRN_V_W = HGRN_HEADS * HGRN_DV

N_MEM = 256
XA_HEADS = 4
XA_DH = D_MODEL // XA_HEADS

N_GROUPS = 4
EXP_PER_GROUP = 8
N_EXPERTS = N_GROUPS * EXP_PER_GROUP
TOP_K = 2
D_FF_EXPERT = 512
MOE_BLOCK = 256

IN_SPLITS = (2 * GDN_QK_W + GDN_V_W, GDN_HEADS, GDN_HEADS, GDN_V_W,
             HGRN_K_W, HGRN_K_W, HGRN_V_W, HGRN_V_W, D_MODEL, D_MODEL)
IN_COLS = 2 * GDN_QK_W + 2 * GDN_V_W + 2 * GDN_HEADS + 2 * HGRN_K_W + 2 * HGRN_V_W + 2 * D_MODEL

kernel_name = 'hybrid_gdn_hgrn2_xattn_hmoe'


def rmsnorm(x, w):
    xf = x.astype(jnp.float32)
    xf = xf * lax.rsqrt(jnp.mean(xf * xf, axis=-1, keepdims=True) + EPS)
    return (xf * w.astype(jnp.float32)).astype(x.dtype)


def l2norm(x):
    xf = x.astype(jnp.float32)
    return (xf * lax.rsqrt(jnp.sum(xf * xf, axis=-1, keepdims=True) + EPS)).astype(x.dtype)


def split_cols(t, sizes):
    out, start = [], 0
    for n in sizes:
        out.append(t[..., start:start + n])
        start += n
    return out


def causal_depthwise_conv(x, w):
    k, c = w.shape
    return lax.conv_general_dilated(
        x, w[:, None, :].astype(x.dtype), window_strides=(1,), padding=[(k - 1, 0)],
        dimension_numbers=('NWC', 'WIO', 'NWC'), feature_group_count=c)


def to_chunks(t):
    b, s, h, d = t.shape
    return t.reshape(b, s // CHUNK, CHUNK, h, d).transpose(0, 3, 1, 2, 4)


def scalar_chunks(t):
    b, s, h = t.shape
    return t.reshape(b, s // CHUNK, CHUNK, h).transpose(0, 3, 1, 2)


def from_chunks(t):
    b, h, n, c, d = t.shape
    return t.transpose(0, 2, 3, 1, 4).reshape(b, n * c, h, d)


def gated_delta_rule(q, k, v, g, beta):
    c = q.shape[-2]
    dk = q.shape[-1]
    dv = v.shape[-1]
    q = q * dk ** -0.5
    causal = jnp.tril(jnp.ones((c, c), dtype=bool))
    strict = jnp.tril(jnp.ones((c, c), dtype=bool), -1)
    cum = jnp.cumsum(g, axis=-1)
    decay = jnp.exp(jnp.where(causal, cum[..., :, None] - cum[..., None, :], -jnp.inf))
    kb = k * beta[..., None]
    a_low = jnp.where(strict, jnp.einsum('bhnid,bhnjd->bhnij', kb, k) * decay, 0.0)
    eye = jnp.eye(c, dtype=q.dtype)
    rhs = jnp.concatenate([v * beta[..., None], kb * jnp.exp(cum)[..., None]], axis=-1)
    sol = lax.linalg.triangular_solve(eye + a_low, rhs, left_side=True, lower=True)
    u, w = sol[..., :dv], sol[..., dv:]
    qk_intra = jnp.where(causal, jnp.einsum('bhnid,bhnjd->bhnij', q, k) * decay, 0.0)
    q_dec = q * jnp.exp(cum)[..., None]
    last = cum[..., -1:]
    k_dec = k * jnp.exp(last - cum)[..., None]
    chunk_decay = jnp.exp(last[..., 0])

    def step(state, xs):
        qi, wi, ui, ki, ai, di = xs
        v_new = ui - jnp.einsum('bhcd,bhde->bhce', wi, state)
        o = jnp.einsum('bhcd,bhde->bhce', qi, state) + jnp.einsum('bhij,bhje->bhie', ai, v_new)
        state = state * di[..., None, None] + jnp.einsum('bhcd,bhce->bhde', ki, v_new)
        return state, o

    b, h = q.shape[0], q.shape[1]
    s0 = jnp.zeros((b, h, dk, dv), q.dtype)
    xs = tuple(jnp.moveaxis(t, 2, 0) for t in (q_dec, w, u, k_dec, qk_intra, chunk_decay))
    _, o = lax.scan(step, s0, xs)
    return jnp.moveaxis(o, 0, 2)


def hgrn2_chunked(q, k, v, log_f):
    c = q.shape[-2]
    causal = jnp.tril(jnp.ones((c, c), dtype=bool))
    cum = jnp.cumsum(log_f, axis=-2)
    q_in = q * jnp.exp(cum)
    k_in = k * jnp.exp(-cum)
    intra = jnp.where(causal, jnp.einsum('bhnid,bhnjd->bhnij', q_in, k_in), 0.0)
    last = cum[..., -1:, :]
    k_dec = k * jnp.exp(last - cum)
    chunk_decay = jnp.exp(last[..., 0, :])

    def step(state, xs):
        qi, ki, vi, ai, di = xs
        o = jnp.einsum('bhcd,bhde->bhce', qi, state) + jnp.einsum('bhij,bhje->bhie', ai, vi)
        state = di[..., :, None] * state + jnp.einsum('bhcd,bhce->bhde', ki, vi)
        return state, o

    b, h, _, _, dk = q.shape
    dv = v.shape[-1]
    s0 = jnp.zeros((b, h, dk, dv), q.dtype)
    xs = tuple(jnp.moveaxis(t, 2, 0) for t in (q_in, k_dec, v, intra, chunk_decay))
    _, o = lax.scan(step, s0, xs)
    return jnp.moveaxis(o, 0, 2)


def mixer_block(hn, w_in, conv_w, a_log, dt_bias, gdn_norm_w, lb, hgrn_norm_w,
                w_branch_a, w_branch_b, w_out):
    b, s, _ = hn.shape
    dt = hn.dtype
    f32 = jnp.float32
    (qkv_a, alpha_pre, beta_pre, og_a, f_pre, q_b, i_b, og_b, gate_a, gate_b) = split_cols(hn @ w_in, IN_SPLITS)

    qkv = jax.nn.silu(causal_depthwise_conv(qkv_a, conv_w))
    q_a = l2norm(qkv[..., :GDN_QK_W].reshape(b, s, GDN_HEADS, GDN_DK))
    k_a = l2norm(qkv[..., GDN_QK_W:2 * GDN_QK_W].reshape(b, s, GDN_HEADS, GDN_DK))
    v_a = qkv[..., 2 * GDN_QK_W:].reshape(b, s, GDN_HEADS, GDN_DV)
    beta = jax.nn.sigmoid(beta_pre.astype(f32))
    log_alpha = -jnp.exp(a_log.astype(f32)) * jax.nn.softplus(alpha_pre.astype(f32) + dt_bias.astype(f32))
    o_a = gated_delta_rule(to_chunks(q_a).astype(f32), to_chunks(k_a).astype(f32), to_chunks(v_a).astype(f32),
                           scalar_chunks(log_alpha), scalar_chunks(beta))
    o_a = from_chunks(o_a).astype(dt)
    y_a = (rmsnorm(o_a, gdn_norm_w) * jax.nn.silu(og_a.reshape(b, s, GDN_HEADS, GDN_DV))).reshape(b, s, GDN_V_W)

    forget = lb + (1.0 - lb) * jax.nn.sigmoid(f_pre.astype(f32))
    log_f = jnp.log(forget)
    k_b = 1.0 - forget
    q_b = q_b.astype(f32) * HGRN_DK ** -0.5
    o_b = hgrn2_chunked(to_chunks(q_b.reshape(b, s, HGRN_HEADS, HGRN_DK)),
                        to_chunks(k_b.reshape(b, s, HGRN_HEADS, HGRN_DK)),
                        to_chunks(i_b.astype(f32).reshape(b, s, HGRN_HEADS, HGRN_DV)),
                        to_chunks(log_f.reshape(b, s, HGRN_HEADS, HGRN_DK)))
    o_b = from_chunks(o_b).astype(dt)
    y_b = (rmsnorm(o_b, hgrn_norm_w) * jax.nn.silu(og_b.reshape(b, s, HGRN_HEADS, HGRN_DV))).reshape(b, s, HGRN_V_W)

    merged = jax.nn.sigmoid(gate_a) * (y_a @ w_branch_a) + jax.nn.sigmoid(gate_b) * (y_b @ w_branch_b)
    return merged @ w_out


def cross_attention(hn, memn, wq, wkv, wo):
    b, s, _ = hn.shape
    m = memn.shape[1]
    q = (hn @ wq).reshape(b, s, XA_HEADS, XA_DH)
    kv = memn @ wkv
    k = kv[..., :D_MODEL].reshape(b, m, XA_HEADS, XA_DH)
    v = kv[..., D_MODEL:].reshape(b, m, XA_HEADS, XA_DH)
    scores = jnp.einsum('bshd,bmhd->bhsm', q, k).astype(jnp.float32) * XA_DH ** -0.5
    p = jax.nn.softmax(scores, axis=-1).astype(v.dtype)
    o = jnp.einsum('bhsm,bmhd->bshd', p, v).reshape(b, s, D_MODEL)
    return o @ wo


def hier_moe(hn, wrg, brg, wre, bre, w_gate, w_up, w_down):
    b, s, d = hn.shape
    t = hn.reshape(-1, d)
    n_tok = t.shape[0]
    g_prob = jax.nn.softmax((t @ wrg + brg).astype(jnp.float32), axis=-1)
    g_p, g_idx = lax.top_k(g_prob, 1)
    e_logits = (t @ wre + bre).astype(jnp.float32).reshape(n_tok, N_GROUPS, EXP_PER_GROUP)
    e_logits = jnp.take_along_axis(e_logits, g_idx[:, :, None], axis=1)[:, 0]
    e_p, e_idx = lax.top_k(jax.nn.softmax(e_logits, axis=-1), TOP_K)
    weights = g_p * e_p / jnp.sum(e_p, axis=-1, keepdims=True)
    expert = g_idx * EXP_PER_GROUP + e_idx

    m = n_tok * TOP_K
    flat_e = expert.reshape(-1).astype(jnp.int32)
    flat_tok = jnp.repeat(jnp.arange(n_tok, dtype=jnp.int32), TOP_K)
    flat_w = weights.reshape(-1)
    order = jnp.argsort(flat_e)
    se, stok, sw = flat_e[order], flat_tok[order], flat_w[order]
    sizes = jnp.bincount(se, length=N_EXPERTS).astype(jnp.int32)
    padded = ((sizes + MOE_BLOCK - 1) // MOE_BLOCK) * MOE_BLOCK
    starts = jnp.cumsum(sizes) - sizes
    pend = jnp.cumsum(padded)
    pstart = pend - padded
    dest = pstart[se] + (jnp.arange(m, dtype=jnp.int32) - starts[se])
    n_rows = ((m + MOE_BLOCK - 1) // MOE_BLOCK) * MOE_BLOCK + N_EXPERTS * MOE_BLOCK
    n_blocks = n_rows // MOE_BLOCK
    x_pad = jnp.zeros((n_rows, d), t.dtype).at[dest].set(t[stok])
    w_pad = jnp.zeros((n_rows,), jnp.float32).at[dest].set(sw)
    tok_pad = jnp.full((n_rows,), n_tok, jnp.int32).at[dest].set(stok)
    blk_start = jnp.arange(n_blocks, dtype=jnp.int32) * MOE_BLOCK
    blk_e = jnp.clip(jnp.searchsorted(pend, blk_start, side='right'), 0, N_EXPERTS - 1)

    def block_mlp(args):
        xb, e = args
        return (jax.nn.silu(xb @ w_gate[e]) * (xb @ w_up[e])) @ w_down[e]

    y_pad = lax.map(block_mlp, (x_pad.reshape(n_blocks, MOE_BLOCK, d), blk_e)).reshape(n_rows, d)
    y_pad = y_pad * w_pad[:, None].astype(y_pad.dtype)
    out = jax.ops.segment_sum(y_pad, tok_pad, num_segments=n_tok)
    return out.reshape(b, s, d)


def setup_inputs(seed: int = 0) -> dict:
    key = jax.random.key(seed)
    ks = jax.random.split(key, 32)
    f32 = jnp.float32
    L = DEPTH

    def nrm(k, shape, fan_in):
        return jax.random.normal(k, shape, f32) * fan_in ** -0.5

    def gain(k, shape):
        return 1.0 + 0.02 * jax.random.normal(k, shape, f32)

    dt_init = jnp.exp(jax.random.uniform(ks[6], (L, GDN_HEADS), f32, math.log(1e-3), math.log(1e-1)))
    return {
        'x': jax.random.normal(ks[0], (BATCH, SEQ, D_MODEL), f32),
        'mem': jax.random.normal(ks[1], (BATCH, N_MEM, D_MODEL), f32),
        'norm_mix_w': gain(ks[2], (L, D_MODEL)),
        'w_in': nrm(ks[3], (L, D_MODEL, IN_COLS), D_MODEL),
        'conv_w': nrm(ks[4], (L, CONV_K, 2 * GDN_QK_W + GDN_V_W), CONV_K),
        'gdn_a_log': jnp.log(jax.random.uniform(ks[5], (L, GDN_HEADS), f32, 1.0, 16.0)),
        'gdn_dt_bias': jnp.log(jnp.expm1(dt_init)),
        'gdn_out_norm_w': gain(ks[7], (L, GDN_DV)),
        'hgrn_lb': 0.1 * jax.random.normal(ks[8], (L + 1, HGRN_K_W), f32),
        'hgrn_out_norm_w': gain(ks[9], (L, HGRN_DV)),
        'w_branch_a': nrm(ks[10], (L, GDN_V_W, D_MODEL), GDN_V_W),
        'w_branch_b': nrm(ks[11], (L, HGRN_V_W, D_MODEL), HGRN_V_W),
        'w_out': nrm(ks[12], (L, D_MODEL, D_MODEL), D_MODEL),
        'norm_xattn_w': gain(ks[13], (L, D_MODEL)),
        'norm_mem_w': gain(ks[14], (L, D_MODEL)),
        'xattn_wq': nrm(ks[15], (L, D_MODEL, D_MODEL), D_MODEL),
        'xattn_wkv': nrm(ks[16], (L, D_MODEL, 2 * D_MODEL), D_MODEL),
        'xattn_wo': nrm(ks[17], (L, D_MODEL, D_MODEL), D_MODEL),
        'norm_ffn_w': gain(ks[18], (L, D_MODEL)),
        'router_group_w': nrm(ks[19], (L, D_MODEL, N_GROUPS), D_MODEL),
        'router_group_b': 0.01 * jax.random.normal(ks[20], (L, N_GROUPS), f32),
        'router_expert_w': nrm(ks[21], (L, D_MODEL, N_EXPERTS), D_MODEL),
        'router_expert_b': 0.01 * jax.random.normal(ks[22], (L, N_EXPERTS), f32),
        'expert_w_gate': nrm(ks[23], (L, N_EXPERTS, D_MODEL, D_FF_EXPERT), D_MODEL),
        'expert_w_up': nrm(ks[24], (L, N_EXPERTS, D_MODEL, D_FF_EXPERT), D_MODEL),
        'expert_w_down': nrm(ks[25], (L, N_EXPERTS, D_FF_EXPERT, D_MODEL), D_FF_EXPERT),
        'final_norm_w': gain(ks[26], (D_MODEL,)),
    }


def reference(x, mem, norm_mix_w, w_in, conv_w, gdn_a_log, gdn_dt_bias, gdn_out_norm_w,
              hgrn_lb, hgrn_out_norm_w, w_branch_a, w_branch_b, w_out,
              norm_xattn_w, norm_mem_w, xattn_wq, xattn_wkv, xattn_wo,
              norm_ffn_w, router_group_w, router_group_b, router_expert_w, router_expert_b,
              expert_w_gate, expert_w_up, expert_w_down, final_norm_w):
    lb_all = jnp.cumsum(jax.nn.softmax(hgrn_lb.astype(jnp.float32), axis=0), axis=0)
    h = x
    for layer in range(DEPTH):
        hn = rmsnorm(h, norm_mix_w[layer])
        h = h + mixer_block(hn, w_in[layer], conv_w[layer], gdn_a_log[layer], gdn_dt_bias[layer],
                            gdn_out_norm_w[layer], lb_all[layer], hgrn_out_norm_w[layer],
                            w_branch_a[layer], w_branch_b[layer], w_out[layer])
        hn = rmsnorm(h, norm_xattn_w[layer])
        memn = rmsnorm(mem, norm_mem_w[layer])
        h = h + cross_attention(hn, memn, xattn_wq[layer], xattn_wkv[layer], xattn_wo[layer])
        hn = rmsnorm(h, norm_ffn_w[layer])
        h = h + hier_moe(hn, router_group_w[layer], router_group_b[layer], router_expert_w[layer],
                         router_expert_b[layer], expert_w_gate[layer], expert_w_up[layer], expert_w_down[layer])
    return rmsnorm(h, final_norm_w)
```

```python
import os
import numpy as np
import ml_dtypes
from contextlib import ExitStack
import concourse.bass as bass
import concourse.mybir as mybir
from concourse.bass_utils import run_bass_kernel_spmd

F32 = mybir.dt.float32
F32R = mybir.dt.float32r
BF16 = mybir.dt.bfloat16
AF = mybir.ActivationFunctionType
ALU = mybir.AluOpType
AX = mybir.AxisListType

S = 4096
D = 1024
NT = S // 128
EPS = 1e-6
NE = 32
DFF = 512
NMEM = 256
INC = 10256
CAP = 512
NSLOT = NE * CAP
BIG = 65536.0
I32 = mybir.dt.int32


class Tok:
    __slots__ = ("w", "r", "nowaw")

    def __init__(self, nowaw=False):
        self.w = {}
        self.r = {}
        self.nowaw = nowaw


class Slot:
    def __init__(self, key, sem):
        self.key = key
        self.sem = sem
        self.cum = 0


class KB:
    def __init__(self, nc, es):
        self.nc = nc
        self.es = es
        self.E = {"pe": nc.tensor, "act": nc.scalar, "dve": nc.vector, "pool": nc.gpsimd, "sp": nc.sync}
        self.semh = {}
        self.cnt = {}
        self.known = {e: {} for e in self.E}
        for e in ("pe", "act", "dve", "pool"):
            self.semh[e] = es.enter_context(nc.semaphore("s_" + e))
            self.cnt[e] = 0
        self.slots = {}
        self.free_slots = []
        self.slot_by_key = {}
        self.n_inst = 0

    def slot(self, name):
        if name not in self.slots:
            if self.free_slots:
                self.slots[name] = self.free_slots.pop()
            else:
                key = "d_%d" % len(self.slot_by_key)
                sem = self.es.enter_context(self.nc.semaphore(key))
                self.semh[key] = sem
                sl = Slot(key, sem)
                self.slot_by_key[key] = sl
                self.slots[name] = sl
        return self.slots[name]

    def _emit_waits(self, e, reads, writes, skip_w_key=None):
        need = {}
        for t in reads:
            for k, v in t.w.items():
                if need.get(k, 0) < v:
                    need[k] = v
        for t in writes:
            if t.nowaw:
                continue
            for k, v in t.w.items():
                if k == skip_w_key:
                    continue
                if need.get(k, 0) < v:
                    need[k] = v
            for k, v in t.r.items():
                if need.get(k, 0) < v:
                    need[k] = v
        kn = self.known[e]
        for k, v in need.items():
            if e == "pe" and k == "pe":
                continue
            if kn.get(k, 0) >= v:
                continue
            self.E[e].wait_ge(self.semh[k], v)
            kn[k] = v

    def op(self, e, fn, reads=(), writes=()):
        self._emit_waits(e, reads, writes)
        inst = fn(self.E[e])
        self.cnt[e] += 1
        c = self.cnt[e]
        inst.then_inc(self.semh[e], 1)
        for t in reads:
            t.r[e] = c
        for t in writes:
            t.w[e] = c
        self.n_inst += 1
        return inst

    def dma(self, q, out, in_, slot, reads=(), writes=(), **kw):
        self._emit_waits(q, reads, writes, skip_w_key=slot.key)
        inst = self.E[q].dma_start(out=out, in_=in_, **kw)
        inst.then_inc(slot.sem, 16)
        slot.cum += 16
        for t in reads:
            t.r[slot.key] = slot.cum
        for t in writes:
            t.w[slot.key] = slot.cum
        self.n_inst += 1
        return inst

    def idma(self, out, out_offset, in_, in_offset, slot, reads=(), writes=(), **kw):
        self._emit_waits("pool", reads, writes, skip_w_key=slot.key)
        inst = self.E["pool"].indirect_dma_start(out=out, out_offset=out_offset, in_=in_, in_offset=in_offset, **kw)
        inst.then_inc(slot.sem, 16)
        slot.cum += 16
        for t in reads:
            t.r[slot.key] = slot.cum
        for t in writes:
            t.w[slot.key] = slot.cum
        self.n_inst += 1
        return inst

    def barrier(self):
        for e in self.E:
            kn = self.known[e]
            for k, sem in self.semh.items():
                v = self.cnt[k] if k in self.cnt else self.slot_by_key[k].cum
                if v == 0 or kn.get(k, 0) >= v:
                    continue
                self.E[e].wait_ge(sem, v)
                kn[k] = v
        self.free_slots.extend(self.slots.values())
        self.slots = {}

    def mm(self, out, lhsT, rhs, start, stop, reads, writes):
        return self.op("pe", lambda e: e.matmul(out, lhsT, rhs, start=start, stop=stop), reads, writes)

    def tr(self, out, in_, ident, reads, writes):
        return self.op("pe", lambda e: e.transpose(out, in_, ident), reads, writes)

    def act(self, out, in_, func, reads, writes, **kw):
        return self.op("act", lambda e: e.activation(out=out, in_=in_, func=func, **kw), reads, writes)

    def tt(self, eng, out, in0, in1, op, reads, writes):
        return self.op(eng, lambda e: e.tensor_tensor(out=out, in0=in0, in1=in1, op=op), reads, writes)

    def ts(self, eng, out, in0, s1, s2, op0, op1, reads, writes):
        if op1 is None:
            return self.op(eng, lambda e: e.tensor_scalar(out=out, in0=in0, scalar1=s1, scalar2=None, op0=op0), reads, writes)
        return self.op(eng, lambda e: e.tensor_scalar(out=out, in0=in0, scalar1=s1, scalar2=s2, op0=op0, op1=op1), reads, writes)

    def stt(self, eng, out, in0, scalar, in1, op0, op1, reads, writes):
        return self.op(eng, lambda e: e.scalar_tensor_tensor(out=out, in0=in0, scalar=scalar, in1=in1, op0=op0, op1=op1), reads, writes)

    def cp(self, eng, out, in_, reads, writes):
        if eng == "act":
            return self.act(out, in_, AF.Copy, reads, writes)
        return self.op(eng, lambda e: e.tensor_copy(out=out, in_=in_), reads, writes)


def run_lockstep2(gens):
    gens = list(gens)
    while gens:
        for gg in list(gens):
            try:
                next(gg)
            except StopIteration:
                gens.remove(gg)


def run_skewed(gen_iter, width):
    it = iter(gen_iter)
    active = []
    exhausted = False
    while True:
        if not exhausted and len(active) < width:
            try:
                active.append(next(it))
            except StopIteration:
                exhausted = True
        if not active:
            if exhausted:
                break
            continue
        for g in list(active):
            try:
                next(g)
            except StopIteration:
                active.remove(g)


class PsumPool:
    def __init__(self, K, nc, es, n=8):
        self.banks = [es.enter_context(nc.psum_tensor("psb%d" % i, [128, 512], F32)) for i in range(n)]
        self.toks = [Tok() for _ in range(n)]
        self.i = 0
        self.n = n
        self.held = set()

    def get(self, hold=False):
        for _ in range(self.n):
            j = self.i
            self.i = (self.i + 1) % self.n
            if j not in self.held:
                if hold:
                    self.held.add(j)
                return self.banks[j], self.toks[j]
        raise RuntimeError("all PSUM banks held")

    def rel(self, bank):
        for j, b in enumerate(self.banks):
            if b is bank:
                self.held.discard(j)
                return


class Rot:
    def __init__(self, nc, es, name, shape, dtype, n):
        self.bufs = [es.enter_context(nc.sbuf_tensor("%s%d" % (name, i), shape, dtype)) for i in range(n)]
        self.toks = [Tok() for _ in range(n)]
        self.i = 0
        self.n = n

    def get(self):
        b, t = self.bufs[self.i], self.toks[self.i]
        self.i = (self.i + 1) % self.n
        return b, t


def host_rmask():
    t = np.arange(S)
    return np.broadcast_to(((t % 64) != 0).astype(np.float32)[None, :], (128, S)).copy()


def host_consts():
    c = {}
    c["identf"] = np.eye(128, dtype=np.float32)
    c["identb"] = np.eye(128, dtype=np.float32).astype(ml_dtypes.bfloat16)
    c["onesb"] = np.ones((128, 128), np.float32).astype(ml_dtypes.bfloat16)
    sel = np.zeros((8, 8, 128), np.float32)
    for h in range(8):
        sel[h, h, :] = 1.0
    c["selh"] = sel
    c["nselh"] = -sel
    i = np.arange(128)[:, None]
    j = np.arange(128)[None, :]
    same = (i // 64) == (j // 64)
    NEG = -30000.0
    c["mnegS"] = np.where(same & (i > j), 0.0, -NEG).astype(np.float32)
    c["mnegIT"] = np.where(same & (j >= i), 0.0, NEG).astype(np.float32)
    jj = (np.arange(128) % 64)[:, None]
    ii = np.arange(64)[None, :]
    c["maskT64"] = (ii >= jj).astype(np.float32)
    c["epsc"] = np.full((128, 1), EPS, np.float32)
    c["UT"] = (np.arange(128)[:, None] < np.arange(128)[None, :]).astype(np.float32).astype(ml_dtypes.bfloat16)
    c["ecap"] = np.broadcast_to((np.arange(NE) * CAP).astype(np.float32)[None, :], (128, NE)).copy()
    c["tokb"] = np.arange(128, dtype=np.float32).reshape(128, 1)
    c["tokall"] = (np.arange(128)[:, None] + 128 * np.arange(NT)[None, :]).astype(np.float32)
    c["onec"] = np.ones((128, 1), np.float32)
    return c


CONST_DT = {"identb": BF16, "onesb": BF16, "UT": BF16}


class Cn:
    pass


class _Stop(Exception):
    pass


def build(dbg=(), stop=None):
    nc = bass.Bass("TRN2", target_bir_lowering=False)
    es = ExitStack()
    K = KB(nc, es)
    try:
        _build(nc, es, K, dbg, stop)
    except _Stop:
        K.barrier()
        return nc
    K.barrier()
    es.close()
    return nc


def _build(nc, es, K, dbg, stop):

    def ext_in(name, shape, dt=F32):
        return nc.dram_tensor(name, list(shape), dt, kind="ExternalInput").ap()

    def scratch(name, shape, dt):
        kind = "ExternalOutput" if name in dbg else "Internal"
        return nc.dram_tensor(name, list(shape), dt, kind=kind).ap()

    x_d = ext_in("x", [S, D])
    mem_d = ext_in("mem", [NMEM, D])
    w_in_d = ext_in("w_in", [D, INC])
    cw_d = ext_in("cw", [128, 24, 4])
    alog_d = ext_in("a_log", [8, 1])
    dtb_d = ext_in("dt_bias", [8, 1])
    gnw_d = ext_in("gdn_nw", [1, 128])
    hnw_d = ext_in("hgrn_nw", [1, 128])
    lb_d = ext_in("lbT", [128, 2, 8])
    wa_d = ext_in("w_a", [D, D])
    wb_d = ext_in("w_b", [D, D])
    wout_d = ext_in("w_out", [D, D])
    wq_d = ext_in("wq", [D, D])
    wkv_d = ext_in("wkv", [D, 2 * D])
    wo_d = ext_in("wo", [D, D])
    nw_d = {n: ext_in(n, [1, D]) for n in ("nw_mix", "nw_xa", "nw_mem", "nw_ffn", "nw_fin")}
    wr_d = ext_in("wr", [D, 36])
    br_d = ext_in("br", [1, 36])
    wg_d = ext_in("wg", [NE, D, DFF])
    wu_d = ext_in("wu", [NE, D, DFF])
    wd_d = ext_in("wd", [NE, DFF, D])
    hc = host_consts()
    cdram = {k: ext_in("c_" + k, v.shape, CONST_DT.get(k, F32)) for k, v in hc.items()}
    rmask_d = ext_in("c_rmask", [128, S])
    out_d = nc.dram_tensor("out", [S, D], F32, kind="ExternalOutput").ap()

    qkvT_s = scratch("qkvT", [3072, S], BF16)
    abT_s = scratch("abT", [16, S], F32)
    ogA_s = scratch("ogA", [S, D], BF16)
    fT_s = scratch("fT", [D, S], BF16)
    qbT_s = scratch("qbT", [D, S], BF16)
    ib_s = scratch("ib", [S, D], BF16)
    ogB_s = scratch("ogB", [S, D], BF16)
    sgAT_s = scratch("sgAT", [D, S], BF16)
    sgBT_s = scratch("sgBT", [D, S], BF16)
    DT = {}
    for n in ("qkvT", "abT", "ogA", "fT", "qbT", "ib", "ogB", "sgAT", "sgBT"):
        DT[n] = Tok(nowaw=True)

    PS = PsumPool(K, nc, es)
    C = Cn()
    ctok = Tok()
    cs = K.slot("const")
    for k, v in hc.items():
        t = es.enter_context(nc.sbuf_tensor("k_" + k, list(v.shape), CONST_DT.get(k, F32)))
        setattr(C, k, t)
        K.dma("sp", t[:], cdram[k], cs, [], [ctok])

    def norm_tile(xt, xtok, wB, wBtok, dst, dsttok, tmp):
        junk, jt = tmp["junk"].get()
        st, stok = tmp["st"].get()
        xs, xst = tmp["xs"].get()
        K.act(junk[:], xt, AF.Square, [xtok], [jt, stok], accum_out=st[:, 0:1])
        K.act(st[:, 1:2], st[:, 0:1], AF.Sqrt, [stok, ctok], [stok], scale=1.0 / D, bias=C.epsc[:, 0:1])
        K.op("dve", lambda e: e.reciprocal(out=st[:, 2:3], in_=st[:, 1:2]), [stok], [stok])
        K.stt("dve", xs[:], xt, st[:, 2:3], wB, ALU.mult, ALU.mult, [xtok, stok, wBtok], [xst])
        ps, pst = PS.get()
        psb = ps[:].bitcast(BF16)
        for kc in range(8):
            K.tr(psb[:, kc * 128:(kc + 1) * 128], xs[:, kc * 128:(kc + 1) * 128], C.identb[:], [xst, ctok], [pst])
        K.cp("act", dst, psb.rearrange("p (k t) -> p k t", k=8), [pst], [dsttok])
        return xs, xst

    def finish():
        raise _Stop()

    with ExitStack() as ph:
        hnT = ph.enter_context(nc.sbuf_tensor("hnT", [128, 8, S], BF16))
        hn_tok = [Tok() for _ in range(NT)]
        if True:
            p1 = ph
            wB = p1.enter_context(nc.sbuf_tensor("wB1", [128, D], F32))
            wBt = Tok()
            K.dma("sp", wB[:], nw_d["nw_mix"].partition_broadcast(128), K.slot("wB"), [], [wBt])
            xr = Rot(nc, p1, "xt", [128, D], F32, 4)
            tmp = {"junk": Rot(nc, p1, "junk", [128, D], BF16, 2), "st": Rot(nc, p1, "st", [128, 4], F32, 4),
                   "xs": Rot(nc, p1, "xs", [128, D], BF16, 2)}
            ndone = [0]
            nload = [0]
            xq = []

            def need(t_hi):
                while ndone[0] < t_hi:
                    while nload[0] < min(NT, ndone[0] + 3):
                        t = nload[0]
                        xt, xtok = xr.get()
                        K.dma("sp", xt[:], x_d[t * 128:(t + 1) * 128, :], K.slot("xt%d" % (t % 4)), [], [xtok])
                        xq.append((xt, xtok))
                        nload[0] += 1
                    t = ndone[0]
                    xt, xtok = xq.pop(0)
                    norm_tile(xt[:], xtok, wB[:], wBt, hnT[:, :, t * 128:(t + 1) * 128], hn_tok[t], tmp)
                    ndone[0] += 1
        if "hnT" in dbg:
            need(NT)
            hd = nc.dram_tensor("hnT_dbg", [128, 8, S], BF16, kind="ExternalOutput").ap()
            K.dma("sp", hd, hnT[:], K.slot("dbg"), hn_tok, [])
        wblk = Rot(nc, ph, "wblk", [128, 8, 512], BF16, 2)
        stgF = Rot(nc, ph, "stgF", [128, S], F32, 2)
        stgB = Rot(nc, ph, "stgB", [128, S], BF16, 2)
        stgT = Rot(nc, ph, "stgT", [128, 512], BF16, 4)
        ev = [0]

        def load_w(c0, n):
            wb_, wt = wblk.get()
            i = wblk.i
            K.dma("pool", wb_[:, :, 0:n], w_in_d[:, c0:c0 + n].rearrange("(kc p) c -> p kc c", p=128),
                  K.slot("wblk%d" % i), [], [wt])
            return wb_, wt

        def fm_block(c0, n, dst, dtok, drow0, func, obf):
            wb_, wt = load_w(c0, n)
            for cc in range((n + 127) // 128):
                m = min(128, n - cc * 128)
                stg, stok = (stgB if obf else stgF).get()
                si = (stgB if obf else stgF).i
                for tt in range(8):
                    need(tt * 4 + 4)
                    ps, pst = PS.get()
                    for kc in range(8):
                        K.mm(ps[0:m, :], wb_[:, kc, cc * 128:cc * 128 + m], hnT[:, kc, tt * 512:(tt + 1) * 512],
                             kc == 0, kc == 7, [wt] + hn_tok[tt * 4:tt * 4 + 4], [pst])
                    if func is not None:
                        K.act(stg[0:m, tt * 512:(tt + 1) * 512], ps[0:m, :], func, [pst], [stok])
                    else:
                        ev[0] ^= 1
                        K.cp("act" if ev[0] else "dve", stg[0:m, tt * 512:(tt + 1) * 512], ps[0:m, :], [pst], [stok])
                K.dma("sp", dst[drow0 + cc * 128:drow0 + cc * 128 + m, :], stg[0:m, :],
                      K.slot(("sB%d" if obf else "sF%d") % si), [stok], [dtok])

        def tm_block(c0, dst, dtok, dcol0, func):
            wb_, wt = load_w(c0, 512)
            for t in range(NT):
                need(t + 1)
                ps, pst = PS.get()
                for kc in range(8):
                    K.mm(ps[:], hnT[:, kc, t * 128:(t + 1) * 128], wb_[:, kc, :], kc == 0, kc == 7,
                         [wt, hn_tok[t]], [pst])
                stg, stok = stgT.get()
                si = stgT.i
                K.act(stg[:], ps[:], func, [pst], [stok])
                K.dma("sp", dst[t * 128:(t + 1) * 128, dcol0:dcol0 + 512], stg[:], K.slot("sT%d" % si), [stok], [dtok])

        for b in range(6):
            fm_block(b * 512, 512, qkvT_s, DT["qkvT"], b * 512, None, True)
        fm_block(3072, 16, abT_s, DT["abT"], 0, None, False)
        for b in range(2):
            tm_block(3088 + b * 512, ogA_s, DT["ogA"], b * 512, AF.Silu)
        for b in range(2):
            fm_block(4112 + b * 512, 512, fT_s, DT["fT"], b * 512, None, True)
        for b in range(2):
            fm_block(5136 + b * 512, 512, qbT_s, DT["qbT"], b * 512, None, True)
        for b in range(2):
            tm_block(6160 + b * 512, ib_s, DT["ib"], b * 512, AF.Copy)
        for b in range(2):
            tm_block(7184 + b * 512, ogB_s, DT["ogB"], b * 512, AF.Silu)
        for b in range(2):
            fm_block(8208 + b * 512, 512, sgAT_s, DT["sgAT"], b * 512, AF.Sigmoid, True)
        for b in range(2):
            fm_block(9232 + b * 512, 512, sgBT_s, DT["sgBT"], b * 512, AF.Sigmoid, True)
        K.barrier()
    if stop == "P2":
        return finish()

    qT_s = scratch("qT", [D, S], BF16)
    qdT_s = scratch("qdT", [D, S], BF16)
    kT_s = scratch("kT", [D, S], BF16)
    kdec_s = scratch("kdec", [S, D], BF16)
    kbdec_s = scratch("kbdec", [S, D], BF16)
    vb_s = scratch("vb", [S, D], BF16)
    yaT_s = scratch("yaT", [D, S], BF16)
    ybT_s = scratch("ybT", [D, S], BF16)
    cd_s = scratch("cd", [8, 64], F32)
    cum_s = scratch("cumrows", [8, S], F32)
    DT["cumrows"] = Tok(nowaw=True)
    oa_s = scratch("o_a", [S, D], F32) if "o_a" in dbg else None
    for n in ("qT", "qdT", "kT", "kdec", "kbdec", "vb", "yaT", "ybT", "cd", "o_a"):
        DT[n] = Tok(nowaw=True)
    DKS = 128 ** -0.5

    with ExitStack() as ph:
        cum = ph.enter_context(nc.sbuf_tensor("g_cum", [8, S], F32))
        cumt = Tok()
        tsc = {n: ph.enter_context(nc.sbuf_tensor("tsc_" + n, [128, NT, 8], F32)) for n in ("elmc", "bec", "be", "nb", "cum")}
        tsct = Tok()
        cdB = ph.enter_context(nc.sbuf_tensor("cdB", [128, 512], F32))
        cdBt = Tok()
        gwB = ph.enter_context(nc.sbuf_tensor("gwB", [128, 128], F32))
        gwt = Tok()
        K.dma("sp", gwB[:], gnw_d.partition_broadcast(128), K.slot("gwB"), [], [gwt])
        cwt = ph.enter_context(nc.sbuf_tensor("g_cw", [128, 24, 4], F32))
        cwtt = Tok()
        K.dma("sp", cwt[:], cw_d, K.slot("cw"), [], [cwtt])
        with ExitStack() as p3:
            ecum = p3.enter_context(nc.sbuf_tensor("g_ecum", [8, S], F32))
            ecumt = Tok()
            with ExitStack() as p3a:
                A = p3a.enter_context(nc.sbuf_tensor("g_A", [8, S], F32))
                Bt = p3a.enter_context(nc.sbuf_tensor("g_B", [8, S], F32))
                Tm = p3a.enter_context(nc.sbuf_tensor("g_T", [8, S], F32))
                rm = p3a.enter_context(nc.sbuf_tensor("g_rm", [8, S], F32))
                sc8 = p3a.enter_context(nc.sbuf_tensor("g_sc8", [8, 4], F32))
                cdr = p3a.enter_context(nc.sbuf_tensor("g_cdr", [8, 64], F32))
                At, Btt, Tmt, rmt, sct, cdrt = Tok(), Tok(), Tok(), Tok(), Tok(), Tok()
                sl = K.slot("g3a")
                K.dma("sp", A[:], abT_s[0:8, :], K.slot("g3a_A"), [DT["abT"]], [At])
                K.dma("sp", Bt[:], abT_s[8:16, :], K.slot("g3a_B"), [DT["abT"]], [Btt])
                K.dma("sp", rm[:], rmask_d[0:8, :], K.slot("g3a_rm"), [], [rmt])
                K.dma("sp", sc8[:, 0:1], alog_d, K.slot("g3a_sc"), [], [sct])
                K.dma("sp", sc8[:, 1:2], dtb_d, K.slot("g3a_sc"), [], [sct])
                K.act(sc8[:, 2:3], sc8[:, 0:1], AF.Exp, [sct], [sct])
                K.ts("dve", sc8[:, 3:4], sc8[:, 2:3], -1.0, None, ALU.mult, None, [sct], [sct])
                K.act(A[:], A[:], AF.Exp, [At, sct], [At], bias=sc8[:, 1:2])
                K.act(A[:], A[:], AF.Ln, [At, ctok], [At], bias=C.onec[0:8, 0:1])
                K.ts("dve", A[:], A[:], sc8[:, 3:4], None, ALU.mult, None, [At, sct], [At])
                K.op("dve", lambda e: e.tensor_tensor_scan(out=cum[:], data0=rm[:], data1=A[:], initial=0.0,
                                                           op0=ALU.mult, op1=ALU.add), [At, rmt], [cumt])
                K.act(Bt[:], Bt[:], AF.Sigmoid, [Btt], [Btt])
                K.act(ecum[:], cum[:], AF.Exp, [cumt], [ecumt])
                cum3 = cum[:].rearrange("p (c t) -> p c t", t=64)
                K.tt("dve", A[:].rearrange("p (c t) -> p c t", t=64), cum3[:, :, 63:64].to_broadcast([8, 64, 64]), cum3,
                     ALU.subtract, [cumt, At], [At])
                K.act(A[:], A[:], AF.Exp, [At], [At])
                K.cp("dve", cdr[:], ecum[:].rearrange("p (c t) -> p c t", t=64)[:, :, 63], [ecumt], [cdrt])
                K.dma("sp", cd_s, cdr[:], K.slot("g3a_cd"), [cdrt], [DT["cd"]])
                K.dma("sp", cdB[:], cd_s.rearrange("h c -> (h c)").partition_broadcast(128), K.slot("g3a_cdB"), [DT["cd"]], [cdBt])

                def to_tok(src, srct, name):
                    ps, pst = PS.get()
                    for b in range(NT):
                        K.tr(ps[:, b * 8:(b + 1) * 8], src[0:8, b * 128:(b + 1) * 128], C.identf[0:8, 0:8], [srct, ctok], [pst])
                    K.cp("act", tsc[name][:].rearrange("p b h -> p (b h)"), ps[:, 0:256], [pst], [tsct])

                to_tok(A, At, "elmc")
                to_tok(cum, cumt, "cum")
                K.dma("sp", cum_s, cum[:], K.slot("g3a_cum"), [cumt], [DT["cumrows"]])
                to_tok(Bt, Btt, "be")
                K.tt("dve", Tm[:], Bt[:], ecum[:], ALU.mult, [Btt, ecumt], [Tmt])
                to_tok(Tm, Tmt, "bec")
                K.ts("dve", Tm[:], Bt[:], -1.0, None, ALU.mult, None, [Btt, Tmt], [Tmt])
                to_tok(Tm, Tmt, "nb")
                K.barrier()
            pre_r = Rot(nc, p3, "g_pre", [128, S + 3], BF16, 3)
            for i_ in range(3):
                K.op("pool", lambda e: e.memset(pre_r.bufs[i_][:, 0:3], 0.0), [], [pre_r.toks[i_]])
            dg_r = Rot(nc, p3, "g_dg", [128, 4, 128], BF16, 3)
            fm_r = Rot(nc, p3, "g_fm", [128, S], BF16, 5)
            tm_r = Rot(nc, p3, "g_tm", [128, NT, 128], BF16, 4)
            sq_r = Rot(nc, p3, "g_sq", [128, 512], BF16, 4)
            sda_r = Rot(nc, p3, "g_sda", [128, S], F32, 2)
            cnt = [0]

            def store_fm(dst, name, h, buf, bt):
                i = [j for j, b_ in enumerate(fm_r.bufs) if b_ is buf][0]
                K.dma("sp", dst[h * 128:(h + 1) * 128, :], buf[:], K.slot("gfm%d" % i), [bt], [DT[name]])

            def store_tm(dst, name, h, buf, bt):
                i = [j for j, b_ in enumerate(tm_r.bufs) if b_ is buf][0]
                for g8 in range(4):
                    K.dma("sp", dst[g8 * 1024:(g8 + 1) * 1024, h * 128:(h + 1) * 128].rearrange("(b p) d -> p b d", p=128),
                          buf[:, g8 * 8:(g8 + 1) * 8, :], K.slot("gtm%d" % i), [bt], [DT[name]])

            def transposes(srcT, srct, outs, h):
                for g8 in range(4):
                    ps, pst = PS.get()
                    psb = ps[:].bitcast(BF16)
                    for j in range(8):
                        b = g8 * 8 + j
                        K.tr(psb[:, j * 128:(j + 1) * 128], srcT[:, b * 128:(b + 1) * 128], C.identb[:], [srct, ctok], [pst])
                    for (nm, buf, bt) in outs:
                        K.tt("dve", buf[:, g8 * 8:(g8 + 1) * 8, :], psb.rearrange("p (b d) -> p b d", b=8),
                             tsc[nm][:, g8 * 8:(g8 + 1) * 8, h:h + 1].to_broadcast([128, 8, 128]), ALU.mult,
                             [pst, tsct], [bt])

            def tile_gen(h, si, tt, pre, pret, dg, dgt, outs, sda, sdat):
                cs_ = slice(tt * 512, (tt + 1) * 512)
                ps, pst = PS.get()
                for j in range(4):
                    K.mm(ps[:], dg[:, j, :], pre[:, tt * 512 + j:tt * 512 + j + 512], j == 0, j == 3, [dgt, pret], [pst])
                xT, xTt = outs[0], outs[1]
                K.act(xT[:, cs_], ps[:], AF.Silu, [pst], [xTt])
                if si == 2:
                    return
                yield
                sq, sqt = sq_r.get()
                K.act(sq[:], xT[:, cs_], AF.Square, [xTt], [sqt])
                ps1, ps1t = PS.get(hold=True)
                K.mm(ps1[:], C.onesb[:], sq[:], True, True, [sqt, ctok], [ps1t])
                yield
                K.ts("dve", sda[:, cs_], ps1[:], EPS, None, ALU.add, None, [ps1t], [sdat])
                PS.rel(ps1)
                yield

            def tail_gen(h, si, outs, sda, sdat):
                for _ in range(4):
                    yield
                if si < 2:
                    K.act(sda[:], sda[:], AF.Ln, [sdat], [sdat])
                    K.act(sda[:], sda[:], AF.Exp, [sdat], [sdat], scale=-0.5)
                    yield
                if si == 0:
                    qT, qTt, qdT, qdTt = outs
                    K.stt("dve", qT[:], qT[:], DKS, sda[:], ALU.mult, ALU.mult, [qTt, sdat], [qTt])
                    for tt in range(8):
                        cs_ = slice(tt * 512, (tt + 1) * 512)
                        ps2, ps2t = PS.get()
                        K.mm(ps2[:], C.selh[0:8, h, :], ecum[0:8, cs_], True, True, [ecumt, ctok], [ps2t])
                        K.tt("dve", qdT[:, cs_], qT[:, cs_], ps2[:], ALU.mult, [qTt, ps2t], [qdTt])
                    store_fm(qT_s, "qT", h, qT, qTt)
                    store_fm(qdT_s, "qdT", h, qdT, qdTt)
                elif si == 1:
                    kT, kTt = outs
                    K.tt("dve", kT[:], kT[:], sda[:], ALU.mult, [kTt, sdat], [kTt])
                    store_fm(kT_s, "kT", h, kT, kTt)
                    yield
                    kd, kdt = tm_r.get()
                    kb, kbt = tm_r.get()
                    transposes(kT, kTt, [("elmc", kd, kdt), ("bec", kb, kbt)], h)
                    store_tm(kdec_s, "kdec", h, kd, kdt)
                    store_tm(kbdec_s, "kbdec", h, kb, kbt)
                else:
                    vT, vTt = outs
                    vbst, vbt = tm_r.get()
                    transposes(vT, vTt, [("be", vbst, vbt)], h)
                    store_tm(vb_s, "vb", h, vbst, vbt)
                yield

            def all_gens():
                for h in range(8):
                    for si in range(3):
                        c = si * 8 + h
                        pre, pret = pre_r.get()
                        K.dma("sp", pre[:, 3:S + 3], qkvT_s[si * 1024 + h * 128: si * 1024 + (h + 1) * 128, :],
                              K.slot("gpre%d" % pre_r.i), [DT["qkvT"]], [pret])
                        dg, dgt = dg_r.get()
                        for j in range(4):
                            K.ts("pool", dg[:, j, :], C.identb[:], cwt[:, c, j:j + 1], None, ALU.mult, None, [ctok, cwtt], [dgt])
                        if si == 0:
                            outs = fm_r.get() + fm_r.get()
                        else:
                            outs = fm_r.get()
                        sda, sdat = sda_r.get() if si < 2 else (None, None)
                        for tt in range(8):
                            yield tile_gen(h, si, tt, pre, pret, dg, dgt, outs, sda, sdat)
                        yield tail_gen(h, si, outs, sda, sdat)

            run_skewed(all_gens(), int(os.environ.get("SKW", "5")))
            K.barrier()
        if stop == "P3":
            raise _Stop()

        def finish_y(px, o_blk, obt, wB_, wt_, og3, ogt_, dst_s, dname, b, R):
            sqj, sqt_ = R["sqj"].get()
            K.tt("pool", sqj[:], o_blk[:], o_blk[:], ALU.mult, [obt], [sqt_])
            ss, sst = R["ss"].get()
            K.op("dve", lambda e: e.tensor_reduce(out=ss[:], in_=sqj[:], axis=AX.X, op=ALU.add), [sqt_], [sst])
            K.act(ss[:], ss[:], AF.Sqrt, [sst, ctok], [sst], scale=1.0 / 128, bias=C.epsc[:, 0:1])
            K.op("dve", lambda e: e.reciprocal(out=ss[:], in_=ss[:]), [sst], [sst])
            K.tt("dve", sqj[:], o_blk[:], ss[:].unsqueeze(2).to_broadcast([128, 8, 128]), ALU.mult, [obt, sst, sqt_], [sqt_])
            K.tt("pool", sqj[:], sqj[:], wB_[:].unsqueeze(1).to_broadcast([128, 8, 128]), ALU.mult, [sqt_, wt_], [sqt_])
            yb, ybt = R["yb"].get()
            K.tt("pool", yb[:], sqj[:], og3, ALU.mult, [sqt_, ogt_], [ybt])
            ps, pst = PS.get()
            psb = ps[:].bitcast(BF16)
            for h in range(8):
                K.tr(psb[:, h * 128:(h + 1) * 128], yb[:, h, :], C.identb[:], [ybt, ctok], [pst])
            yT, yTt = R["yT"].get()
            K.cp("act", yT[:], psb.rearrange("p (h t) -> p h t", h=8), [pst], [yTt])
            K.dma("sp", dst_s[:, b * 128:(b + 1) * 128].rearrange("(h p) t -> p h t", p=128), yT[:],
                  K.slot("%s_yT%d" % (px, R["yT"].i)), [yTt], [DT[dname]])

        with ExitStack() as p4:
            names = ("qT", "qdT", "kT", "kdec", "kbdec", "vb", "ogA")
            ld_r = {n: Rot(nc, p4, "l_" + n, [128, 1024], BF16, 3) for n in names}
            cr_r = Rot(nc, p4, "l_cr", [128, 8, 128], F32, 3)
            dm_r = Rot(nc, p4, "g_dm", [128, 2, 512], F32, 2)
            tmp_r = Rot(nc, p4, "g_tmp", [128, 512], F32, 2)
            n0_r = Rot(nc, p4, "g_n0", [128, 512], F32R, 2)
            n0T_r = Rot(nc, p4, "g_n0T", [128, 512], F32R, 2)
            m_r = Rot(nc, p4, "g_m", [128, 2, 512], F32R, 4)
            t_r = Rot(nc, p4, "g_t", [128, 512], F32R, 4)
            tb_r = Rot(nc, p4, "g_tb", [128, 512], BF16, 4)
            qk_r = Rot(nc, p4, "g_qk", [128, 512], BF16, 4)
            nw_r = Rot(nc, p4, "g_nw", [128, 512], BF16, 4)
            vn_r = Rot(nc, p4, "g_vn", [128, 512], BF16, 2)
            ob_r = Rot(nc, p4, "g_ob", [128, 8, 128], F32, 2)
            YR = {"sqj": Rot(nc, p4, "g_sqj", [128, 8, 128], F32, 1), "ss": Rot(nc, p4, "g_ss", [128, 8], F32, 2),
                  "yb": Rot(nc, p4, "g_yb", [128, 8, 128], BF16, 2), "yT": Rot(nc, p4, "g_yT", [128, 8, 128], BF16, 2)}
            S32 = p4.enter_context(nc.sbuf_tensor("g_S32", [128, 8, 128], F32))
            Sb = p4.enter_context(nc.sbuf_tensor("g_Sb", [128, 8, 128], BF16))
            s32t = [Tok(), Tok()]
            sbt = [Tok(), Tok()]
            for g in range(2):
                K.op("dve", lambda e: e.memset(S32[:, 4 * g:4 * g + 4, :], 0.0), [], [s32t[g]])
                K.op("dve", lambda e: e.memset(Sb[:, 4 * g:4 * g + 4, :], 0.0), [], [sbt[g]])
            cdB3 = cdB[:].rearrange("p (h c) -> p h c", c=64)
            H3 = lambda ap: ap.rearrange("p (h j) -> p h j", h=4)

            def load_block(b):
                bufs = {}
                ldt = Tok()
                bc = slice(b * 128, (b + 1) * 128)
                for n in names:
                    buf, _ = ld_r[n].get()
                    bufs[n] = buf
                i = ld_r["qT"].i
                sl_ = K.slot("gld%d" % i)
                prev = ld_r["qT"].toks[i]
                for n, src in (("qT", qT_s), ("qdT", qdT_s), ("kT", kT_s)):
                    K.dma("sp", bufs[n][:].rearrange("p (h t) -> p h t", h=8), src[:, bc].rearrange("(h p) t -> p h t", p=128),
                          sl_, [DT[n]], [prev])
                for n, src in (("kdec", kdec_s), ("kbdec", kbdec_s), ("vb", vb_s), ("ogA", ogA_s)):
                    K.dma("sp", bufs[n][:], src[bc, :], sl_, [DT[n]], [prev])
                crb, _ = cr_r.get()
                bufs["cr"] = crb
                for h_ in range(8):
                    K.dma("sp", crb[:, h_, :], cum_s[h_:h_ + 1, bc].partition_broadcast(128), sl_, [DT["cumrows"]], [prev])
                return bufs, prev

            def run_lockstep(gens):
                gens = list(gens)
                while gens:
                    for gg in list(gens):
                        try:
                            next(gg)
                        except StopIteration:
                            gens.remove(gg)

            prods = {}

            def front(b, g, L, ldt):
                bc = slice(b * 128, (b + 1) * 128)
                qTb = L["qT"][:].rearrange("p (h t) -> p h t", h=8)
                kTb = L["kT"][:].rearrange("p (h t) -> p h t", h=8)
                kbb = L["kbdec"]
                hs = list(range(4 * g, 4 * g + 4))
                psG, tG = PS.get()
                psQK, tQK = PS.get()
                for hl, h in enumerate(hs):
                    c = slice(hl * 128, (hl + 1) * 128)
                    K.mm(psG[:, c], kTb[:, h, :], kTb[:, h, :], True, True, [ldt], [tG])
                    K.mm(psQK[:, c], kTb[:, h, :], qTb[:, h, :], True, True, [ldt], [tQK])
                dm, dmt = dm_r.get()
                crb = L["cr"]
                for hl, h in enumerate(hs):
                    c = slice(hl * 128, (hl + 1) * 128)
                    K.stt("dve", dm[:, 0, c], crb[:, h, :], tsc["cum"][:, b, h:h + 1], C.mnegS[:], ALU.subtract, ALU.add,
                          [ldt, tsct, ctok], [dmt])
                    K.stt("dve", dm[:, 1, c], crb[:, h, :], tsc["cum"][:, b, h:h + 1], C.mnegIT[:], ALU.subtract, ALU.add,
                          [ldt, tsct, ctok], [dmt])
                K.act(dm[:, 0, :], dm[:, 0, :], AF.Exp, [dmt], [dmt], scale=-1.0)
                K.act(dm[:, 1, :], dm[:, 1, :], AF.Exp, [dmt], [dmt])
                tmpN, tnt = tmp_r.get()
                K.tt("dve", H3(tmpN[:]), H3(psG[:]), tsc["nb"][:, b, 4 * g:4 * g + 4].unsqueeze(2).to_broadcast([128, 4, 128]),
                     ALU.mult, [tG, tsct], [tnt])
                N0, n0t = n0_r.get()
                K.tt("dve", N0[:], tmpN[:], dm[:, 0, :], ALU.mult, [tnt, dmt], [n0t])
                qkT, qkt = qk_r.get()
                K.tt("dve", qkT[:], psQK[:], dm[:, 1, :], ALU.mult, [tQK, dmt], [qkt])
                yield
                psT, tT = PS.get()
                for hl in range(4):
                    c = slice(hl * 128, (hl + 1) * 128)
                    K.tr(psT[:, c], N0[:, c].bitcast(F32), C.identf[:], [n0t, ctok], [tT])
                N0T, n0Tt = n0T_r.get()
                K.cp("act", N0T[:], psT[:], [tT], [n0Tt])
                Tt, ttt = t_r.get()
                K.tt("dve", H3(Tt[:]), H3(N0T[:].bitcast(F32)), C.identf[:].unsqueeze(1).to_broadcast([128, 4, 128]), ALU.add,
                     [n0Tt, ctok], [ttt])
                yield
                M, Mtk, MT, MTtk = N0[:], n0t, N0T[:], n0Tt
                for lvl in range(1, 6):
                    last = (lvl == 5)
                    psM, tM = PS.get()
                    for hl in range(4):
                        c = slice(hl * 128, (hl + 1) * 128)
                        K.mm(psM[:, c], MT[:, c], M[:, c], True, True, [Mtk, MTtk], [tM])
                    if not last:
                        psMT, tMT = PS.get()
                        for hl in range(4):
                            c = slice(hl * 128, (hl + 1) * 128)
                            K.mm(psMT[:, c], M[:, c], MT[:, c], True, True, [Mtk, MTtk], [tMT])
                    Mn, Mnt = m_r.get()
                    K.cp("act", Mn[:, 0, :], psM[:], [tM], [Mnt])
                    if not last:
                        K.cp("act", Mn[:, 1, :], psMT[:], [tMT], [Mnt])
                    yield
                    psP, tP = PS.get()
                    for hl in range(4):
                        c = slice(hl * 128, (hl + 1) * 128)
                        K.mm(psP[:, c], Mn[:, 0, c], Tt[:, c], True, True, [Mnt, ttt], [tP])
                    if not last:
                        Tn, tnn = t_r.get()
                        K.tt("dve", Tn[:], Tt[:].bitcast(F32), psP[:], ALU.add, [ttt, tP], [tnn])
                        Tt, ttt = Tn, tnn
                        M, Mtk, MT, MTtk = Mn[:, 0, :], Mnt, Mn[:, 1, :], Mnt
                    else:
                        Ttb, ttbt = tb_r.get()
                        K.tt("dve", Ttb[:], Tt[:].bitcast(F32), psP[:], ALU.add, [ttt, tP], [ttbt])
                    yield
                psW, tW = PS.get()
                for hl, h in enumerate(hs):
                    c = slice(hl * 128, (hl + 1) * 128)
                    K.mm(psW[:, c], kbb[:, h * 128:(h + 1) * 128], Ttb[:, c], True, True, [ldt, ttbt], [tW])
                nwT, nwt = nw_r.get()
                K.act(nwT[:], psW[:], AF.Copy, [tW], [nwt], scale=-1.0)
                prods[(b, g)] = (Ttb, ttbt, qkT, qkt, nwT, nwt)
                yield

            def back(b, g, L, ldt, o_blk, obt):
                Ttb, ttbt, qkT, qkt, nwT, nwt = prods.pop((b, g))
                qdTb = L["qdT"][:].rearrange("p (h t) -> p h t", h=8)
                kdb, vbb = L["kdec"], L["vb"]
                hs = list(range(4 * g, 4 * g + 4))
                for ci in range(2):
                    r = slice(ci * 64, ci * 64 + 64)
                    psV, tV = PS.get()
                    for hl, h in enumerate(hs):
                        c = slice(hl * 128, (hl + 1) * 128)
                        ic = slice(hl * 128 + ci * 64, hl * 128 + ci * 64 + 64)
                        K.mm(psV[r, c], Ttb[:, ic], vbb[:, h * 128:(h + 1) * 128], True, False, [ttbt, ldt], [tV])
                        K.mm(psV[r, c], nwT[:, ic], Sb[:, h, :], False, True, [nwt, sbt[g]], [tV])
                    vn, vnt = vn_r.get()
                    K.cp("act", vn[r, :], psV[r, :], [tV], [vnt])
                    yield
                    psO, tO = PS.get()
                    for hl, h in enumerate(hs):
                        c = slice(hl * 128, (hl + 1) * 128)
                        ic = slice(hl * 128 + ci * 64, hl * 128 + ci * 64 + 64)
                        K.mm(psO[r, c], qdTb[:, h, ci * 64:ci * 64 + 64], Sb[:, h, :], True, False, [ldt, sbt[g]], [tO])
                        K.mm(psO[r, c], qkT[r, ic], vn[r, c], False, True, [qkt, vnt], [tO])
                    K.cp("act", o_blk[r, 4 * g:4 * g + 4, :], H3(psO[r, :]), [tO], [obt])
                    psS, tS = PS.get()
                    for hl, h in enumerate(hs):
                        c = slice(hl * 128, (hl + 1) * 128)
                        K.mm(psS[:, c], kdb[r, h * 128:(h + 1) * 128], vn[r, c], True, True, [ldt, vnt], [tS])
                    ch = 2 * b + ci
                    Sg = S32[:, 4 * g:4 * g + 4, :]
                    K.tt("dve", Sg, Sg, cdB3[:, 4 * g:4 * g + 4, ch:ch + 1].to_broadcast([128, 4, 128]), ALU.mult,
                         [s32t[g], cdBt], [s32t[g]])
                    K.tt("dve", Sg, Sg, H3(psS[:]), ALU.add, [s32t[g], tS], [s32t[g]])
                    K.cp("act", Sb[:, 4 * g:4 * g + 4, :], Sg, [s32t[g]], [sbt[g]])
                    yield

            blocks = {0: load_block(0)}
            if NT > 1:
                blocks[1] = load_block(1)
            run_lockstep([front(0, 0, *blocks[0]), front(0, 1, *blocks[0])])
            for b in range(NT):
                L, ldt = blocks[b]
                if b + 2 < NT:
                    blocks[b + 2] = load_block(b + 2)
                bc = slice(b * 128, (b + 1) * 128)
                o_blk, obt = ob_r.get()
                gens = [back(b, 0, L, ldt, o_blk, obt)]
                if b + 1 < NT:
                    gens.append(front(b + 1, 0, *blocks[b + 1]))
                gens.append(back(b, 1, L, ldt, o_blk, obt))
                if b + 1 < NT:
                    gens.append(front(b + 1, 1, *blocks[b + 1]))
                run_lockstep(gens)
                if oa_s is not None:
                    K.dma("sp", oa_s[bc, :], o_blk[:].rearrange("p h d -> p (h d)"), K.slot("oa%d" % ob_r.i), [obt], [DT["o_a"]])
                finish_y("a", o_blk, obt, gwB, gwt, L["ogA"][:].rearrange("p (h d) -> p h d", h=8), ldt, yaT_s, "yaT", b, YR)
                del blocks[b]
            K.barrier()
        if stop == "P4":
            raise _Stop()

    lgall = es.enter_context(nc.sbuf_tensor("t_lgall", [128, NT, 36], F32))
    lgt = Tok()
    qinT_s = scratch("qinT", [D, S], BF16)
    kinT_s = scratch("kinT", [D, S], BF16)
    kdb_s = scratch("kdecb", [S, D], BF16)
    ob_s = scratch("o_b", [S, D], F32) if "o_b" in dbg else None
    for n in ("qinT", "kinT", "kdecb", "o_b"):
        DT[n] = Tok(nowaw=True)
    with ExitStack() as ph:
        elast = es.enter_context(nc.sbuf_tensor("h_elast", [128, 8, 64], F32))
        elt = Tok()
        hwB = es.enter_context(nc.sbuf_tensor("hwB", [128, 128], F32))
        hwt = Tok()
        K.dma("sp", hwB[:], hnw_d.partition_broadcast(128), K.slot("hwB"), [], [hwt])
        lbt_ = es.enter_context(nc.sbuf_tensor("h_lbt", [128, 2, 8], F32))
        lb = es.enter_context(nc.sbuf_tensor("h_lb", [128, 8], F32))
        oml = es.enter_context(nc.sbuf_tensor("h_oml", [128, 8], F32))
        lbtok = Tok()
        K.dma("sp", lbt_[:], lb_d, K.slot("lbt"), [], [lbtok])
        K.tt("dve", lb[:], lbt_[:, 0, :], lbt_[:, 1, :], ALU.subtract, [lbtok], [lbtok])
        K.act(lb[:], lb[:], AF.Sigmoid, [lbtok], [lbtok])
        K.ts("dve", oml[:], lb[:], -1.0, 1.0, ALU.mult, ALU.add, [lbtok], [lbtok])
        with ExitStack() as p5:
            PW = 2048
            hrm = p5.enter_context(nc.sbuf_tensor("h_rm", [128, PW], F32))
            hrmt = Tok()
            K.dma("sp", hrm[:], rmask_d[:, 0:PW], K.slot("hrm"), [], [hrmt])
            hf_r = Rot(nc, p5, "h_f", [128, PW], F32, 3)
            hfl_r = Rot(nc, p5, "h_fl", [128, PW], BF16, 3)
            hq_r = Rot(nc, p5, "h_q", [128, PW], BF16, 3)
            h2_r = Rot(nc, p5, "h_b2", [128, PW], F32, 3)
            h3_r = Rot(nc, p5, "h_b3", [128, PW], F32, 3)
            h4_r = Rot(nc, p5, "h_b4", [128, PW], F32, 3)
            hqo_r = Rot(nc, p5, "h_qo", [128, PW], BF16, 3)
            hko_r = Rot(nc, p5, "h_ko", [128, PW], BF16, 3)
            hkd_r = Rot(nc, p5, "h_kd", [128, PW], BF16, 3)
            hkt_r = Rot(nc, p5, "h_kt", [128, PW // 128, 128], BF16, 3)
            hcnt = [0]

            def hslot(px):
                hcnt[0] = (hcnt[0] + 1) % 6
                return K.slot("%s%d" % (px, hcnt[0]))

            def hpiece(h, pc):
                cs_ = slice(pc * PW, (pc + 1) * PW)
                nch = PW // 64
                fl, flt = hfl_r.get()
                K.dma("sp", fl[:], fT_s[h * 128:(h + 1) * 128, cs_], K.slot("hlf%d" % hfl_r.i), [DT["fT"]], [flt])
                qb, qbt = hq_r.get()
                K.dma("sp", qb[:], qbT_s[h * 128:(h + 1) * 128, cs_], K.slot("hlq%d" % hq_r.i), [DT["qbT"]], [qbt])
                yield
                fb, fbt = hf_r.get()
                K.act(fb[:], fl[:], AF.Sigmoid, [flt], [fbt])
                yield
                K.ts("dve", fb[:], fb[:], oml[:, h:h + 1], lb[:, h:h + 1], ALU.mult, ALU.add, [fbt, lbtok], [fbt])
                yield
                b2, b2t = h2_r.get()
                K.act(b2[:], fb[:], AF.Ln, [fbt], [b2t])
                yield
                b3, b3t = h3_r.get()
                K.op("dve", lambda e: e.tensor_tensor_scan(out=b3[:], data0=hrm[:], data1=b2[:], initial=0.0,
                                                           op0=ALU.mult, op1=ALU.add), [b2t, hrmt], [b3t])
                yield
                b4, b4t = h4_r.get()
                K.act(b4[:], b3[:], AF.Exp, [b3t], [b4t])
                K.act(b2[:], b3[:], AF.Exp, [b3t, b2t], [b2t], scale=-1.0)
                yield
                el_ = elast[:, h, pc * nch:(pc + 1) * nch]
                K.cp("dve", el_, b4[:].rearrange("p (c t) -> p c t", t=64)[:, :, 63], [b4t], [elt])
                qin, qint = hqo_r.get()
                K.stt("dve", qin[:], qb[:], DKS, b4[:], ALU.mult, ALU.mult, [qbt, b4t], [qint])
                K.ts("pool", fb[:], fb[:], -1.0, 1.0, ALU.mult, ALU.add, [fbt], [fbt])
                K.tt("pool", b2[:], fb[:], b2[:], ALU.mult, [fbt, b2t], [b2t])
                yield
                kin, kint = hko_r.get()
                K.cp("act", kin[:], b2[:], [b2t], [kint])
                kdT, kdTt = hkd_r.get()
                K.tt("dve", kdT[:].rearrange("p (c t) -> p c t", t=64), b2[:].rearrange("p (c t) -> p c t", t=64),
                     el_.unsqueeze(2).to_broadcast([128, nch, 64]), ALU.mult, [b2t, elt], [kdTt])
                K.dma("sp", qinT_s[h * 128:(h + 1) * 128, cs_], qin[:], K.slot("hsq%d" % hqo_r.i), [qint], [DT["qinT"]])
                K.dma("sp", kinT_s[h * 128:(h + 1) * 128, cs_], kin[:], K.slot("hsk%d" % hko_r.i), [kint], [DT["kinT"]])
                yield
                nb_ = PW // 128
                kd, kdt = hkt_r.get()
                for g8 in range(nb_ // 8):
                    ps, pst = PS.get()
                    psb = ps[:].bitcast(BF16)
                    for j in range(8):
                        bb = g8 * 8 + j
                        K.tr(psb[:, j * 128:(j + 1) * 128], kdT[:, bb * 128:(bb + 1) * 128], C.identb[:], [kdTt, ctok], [pst])
                    K.cp("act", kd[:, g8 * 8:(g8 + 1) * 8, :], psb.rearrange("p (b d) -> p b d", b=8), [pst], [kdt])
                for g8 in range(nb_ // 8):
                    r0 = pc * PW + g8 * 1024
                    K.dma("sp", kdb_s[r0:r0 + 1024, h * 128:(h + 1) * 128].rearrange("(b p) d -> p b d", p=128),
                          kd[:, g8 * 8:(g8 + 1) * 8, :], K.slot("hsd%d" % hkt_r.i), [kdt], [DT["kdecb"]])
                yield

            run_skewed((hpiece(h, pc) for h in range(8) for pc in range(S // PW)), 3)
            K.barrier()
        if stop == "P5":
            raise _Stop()
        wst = ExitStack()

        def res_w(name, src):
            t = wst.enter_context(nc.sbuf_tensor("rw_" + name, [128, 8, D], BF16))
            tk = Tok()
            K.dma("pool", t[:], src.rearrange("(kc p) c -> p kc c", p=128), K.slot("w_" + name), [], [tk])
            return t, tk
        Wa, Wat = res_w("a", wa_d)
        Wb, Wbt = res_w("b", wb_d)
        Wo_, Wot = res_w("out", wout_d)
        Wq, Wqt = res_w("q", wq_d)
        Wxo, Wxot = res_w("xo", wo_d)
        Wr = wst.enter_context(nc.sbuf_tensor("rw_r", [128, 8, 36], BF16))
        Wrt = Tok()
        K.dma("pool", Wr[:], wr_d.rearrange("(kc p) c -> p kc c", p=128), K.slot("w_r"), [], [Wrt])

        with ExitStack() as p6:
            names = ("qinT", "kinT", "kdecb", "ib", "ogB")
            srcs = {"qinT": qinT_s, "kinT": kinT_s, "kdecb": kdb_s, "ib": ib_s, "ogB": ogB_s}
            ld_r = {n: Rot(nc, p6, "hl_" + n, [128, 1024], BF16, 3) for n in names}
            iT_r = Rot(nc, p6, "h_iT", [128, 4, 64], BF16, 6)
            ob_r = Rot(nc, p6, "h_ob", [128, 8, 128], F32, 2)
            YR = {"sqj": Rot(nc, p6, "h_sqj", [128, 8, 128], F32, 1), "ss": Rot(nc, p6, "h_ss", [128, 8], F32, 2),
                  "yb": Rot(nc, p6, "h_yb", [128, 8, 128], BF16, 2), "yT": Rot(nc, p6, "h_yT", [128, 8, 128], BF16, 2)}
            S32 = p6.enter_context(nc.sbuf_tensor("h_S32", [128, 8, 128], F32))
            Sb = p6.enter_context(nc.sbuf_tensor("h_Sb", [128, 8, 128], BF16))
            s32t = [Tok(), Tok()]
            sbt = [Tok(), Tok()]
            for g in range(2):
                K.op("dve", lambda e: e.memset(S32[:, 4 * g:4 * g + 4, :], 0.0), [], [s32t[g]])
                K.op("dve", lambda e: e.memset(Sb[:, 4 * g:4 * g + 4, :], 0.0), [], [sbt[g]])
            H3 = lambda ap: ap.rearrange("p (h j) -> p h j", h=4)

            def load_block(b):
                bufs = {}
                bc = slice(b * 128, (b + 1) * 128)
                for n in names:
                    buf, _ = ld_r[n].get()
                    bufs[n] = buf
                i = ld_r["qinT"].i
                sl_ = K.slot("gld%d" % i)
                prev = ld_r["qinT"].toks[i]
                for n in ("qinT", "kinT"):
                    K.dma("sp", bufs[n][:].rearrange("p (h t) -> p h t", h=8), srcs[n][:, bc].rearrange("(h p) t -> p h t", p=128),
                          sl_, [DT[n]], [prev])
                for n in ("kdecb", "ib", "ogB"):
                    K.dma("sp", bufs[n][:], srcs[n][bc, :], sl_, [DT[n]], [prev])
                return bufs, prev

            nxt = load_block(0)
            for b in range(NT):
                L, ldt = nxt
                if b + 1 < NT:
                    nxt = load_block(b + 1)
                bc = slice(b * 128, (b + 1) * 128)
                qin = L["qinT"][:].rearrange("p (h t) -> p h t", h=8)
                kin = L["kinT"][:].rearrange("p (h t) -> p h t", h=8)
                kdb, ibb = L["kdecb"], L["ib"]
                o_blk, obt = ob_r.get()

                def hg(g):
                    def intra(ci):
                        r = slice(ci * 64, ci * 64 + 64)
                        cc = slice(ci * 64, ci * 64 + 64)
                        psI, tI = PS.get()
                        for hl in range(4):
                            h = 4 * g + hl
                            K.mm(psI[r, hl * 64:(hl + 1) * 64], kin[:, h, cc], qin[:, h, cc], True, True, [ldt], [tI])
                        iT, iTt = iT_r.get()
                        K.tt("dve", iT[r, 0:4, :], psI[r, 0:256].rearrange("p (h i) -> p h i", h=4),
                             C.maskT64[r, :].unsqueeze(1).to_broadcast([64, 4, 64]), ALU.mult, [tI, ctok], [iTt])
                        return iT, iTt
                    cur = intra(0)
                    yield
                    for ci in range(2):
                        r = slice(ci * 64, ci * 64 + 64)
                        cc = slice(ci * 64, ci * 64 + 64)
                        ch = 2 * b + ci
                        iT, iTt = cur
                        psO, tO = PS.get()
                        for hl in range(4):
                            h = 4 * g + hl
                            c = slice(hl * 128, (hl + 1) * 128)
                            K.mm(psO[r, c], qin[:, h, cc], Sb[:, h, :], True, False, [ldt, sbt[g]], [tO])
                            K.mm(psO[r, c], iT[r, hl, :], ibb[r, h * 128:(h + 1) * 128], False, True, [iTt, ldt], [tO])
                        K.cp("act", o_blk[r, 4 * g:4 * g + 4, :], H3(psO[r, :]), [tO], [obt])
                        psS, tS = PS.get()
                        for hl in range(4):
                            h = 4 * g + hl
                            c = slice(hl * 128, (hl + 1) * 128)
                            K.mm(psS[:, c], kdb[r, h * 128:(h + 1) * 128], ibb[r, h * 128:(h + 1) * 128], True, True, [ldt], [tS])
                        Sg = S32[:, 4 * g:4 * g + 4, :]
                        K.tt("dve", Sg, Sg, elast[:, 4 * g:4 * g + 4, ch:ch + 1].to_broadcast([128, 4, 128]), ALU.mult,
                             [s32t[g], elt], [s32t[g]])
                        K.tt("dve", Sg, Sg, H3(psS[:]), ALU.add, [s32t[g], tS], [s32t[g]])
                        K.cp("act", Sb[:, 4 * g:4 * g + 4, :], Sg, [s32t[g]], [sbt[g]])
                        if ci == 0:
                            cur = intra(1)
                        yield

                run_lockstep2([hg(0), hg(1)])
                if ob_s is not None:
                    K.dma("sp", ob_s[bc, :], o_blk[:].rearrange("p h d -> p (h d)"), K.slot("oa%d" % ob_r.i), [obt], [DT["o_b"]])
                finish_y("b", o_blk, obt, hwB, hwt, L["ogB"][:].rearrange("p (h d) -> p h d", h=8), ldt, ybT_s, "ybT", b, YR)
            K.barrier()
    if stop == "P6":
        raise _Stop()

    h2_s = scratch("h2", [S, D], F32)
    hn3T_s = scratch("hn3T", [D, S], BF16) if "hn3T" in dbg else None
    hn3k_s = scratch("hn3k", [S + 1, D], BF16)
    meta_s = scratch("meta", [NSLOT, 3], F32)
    ytok_s = scratch("ytok", [2 * S, D], BF16)
    for n in ("hn3k", "meta", "ytok"):
        DT[n] = Tok(nowaw=True)
    comb_s = scratch("comb", [S, NE], F32)
    h1_s = scratch("h1", [S, D], F32) if "h1" in dbg else None
    for n in ("h2", "hn3T", "comb", "h1"):
        DT[n] = Tok(nowaw=True)
    TT = 256
    with ExitStack() as ph:
        brB = ph.enter_context(nc.sbuf_tensor("brB", [128, 36], F32))
        brt = Tok()
        K.dma("sp", brB[:], br_d.partition_broadcast(128), K.slot("brB"), [], [brt])
        wBx = ph.enter_context(nc.sbuf_tensor("wBx", [128, D], F32))
        wBf = ph.enter_context(nc.sbuf_tensor("wBf", [128, D], F32))
        wBxt, wBft = Tok(), Tok()
        K.dma("sp", wBx[:], nw_d["nw_xa"].partition_broadcast(128), K.slot("wBx"), [], [wBxt])
        K.dma("sp", wBf[:], nw_d["nw_ffn"].partition_broadcast(128), K.slot("wBf"), [], [wBft])
        KT = ph.enter_context(nc.sbuf_tensor("x_KT", [128, 8, NMEM], BF16))
        V = ph.enter_context(nc.sbuf_tensor("x_V", [128, 2, D], BF16))
        KTt, Vt = Tok(), Tok()
        tmp = {"junk": Rot(nc, ph, "n_junk", [128, D], BF16, 1), "st": Rot(nc, ph, "n_st", [128, 4], F32, 4),
               "xs": Rot(nc, ph, "n_xs", [128, D], BF16, 2)}
        with ExitStack() as pm:
            wBm = pm.enter_context(nc.sbuf_tensor("wBm", [128, D], F32))
            wBmt = Tok()
            K.dma("sp", wBm[:], nw_d["nw_mem"].partition_broadcast(128), K.slot("wBm"), [], [wBmt])
            memnT = pm.enter_context(nc.sbuf_tensor("memnT", [128, 8, NMEM], BF16))
            mnt = Tok()
            mr = Rot(nc, pm, "m_x", [128, D], F32, 2)
            wkr = Rot(nc, pm, "m_w", [128, 8, 512], BF16, 2)
            for mt in range(2):
                xt, xtok = mr.get()
                K.dma("sp", xt[:], mem_d[mt * 128:(mt + 1) * 128, :], K.slot("xt%d" % mt), [], [xtok])
                norm_tile(xt[:], xtok, wBm[:], wBmt, memnT[:, :, mt * 128:(mt + 1) * 128], mnt, tmp)
            for blk in range(4):
                wk, wkt = wkr.get()
                K.dma("pool", wk[:], wkv_d[:, blk * 512:(blk + 1) * 512].rearrange("(kc p) c -> p kc c", p=128),
                      K.slot("wblk%d" % wkr.i), [], [wkt])
                if blk < 2:
                    for cc in range(4):
                        dc = blk * 4 + cc
                        ps, pst = PS.get()
                        for kc in range(8):
                            K.mm(ps[:, 0:NMEM], wk[:, kc, cc * 128:(cc + 1) * 128], memnT[:, kc, :], kc == 0, kc == 7, [wkt, mnt], [pst])
                        K.cp("act", KT[:, dc, :], ps[:, 0:NMEM], [pst], [KTt])
                else:
                    for mt in range(2):
                        ps, pst = PS.get()
                        for kc in range(8):
                            K.mm(ps[:], memnT[:, kc, mt * 128:(mt + 1) * 128], wk[:, kc, :], kc == 0, kc == 7, [wkt, mnt], [pst])
                        K.cp("act", V[:, mt, (blk - 2) * 512:(blk - 1) * 512], ps[:], [pst], [Vt])
            K.barrier()
        NS = TT // 128
        ldn = ("yaT", "ybT", "sgAT", "sgBT")
        lsrc = {"yaT": yaT_s, "ybT": ybT_s, "sgAT": sgAT_s, "sgBT": sgBT_s}
        ld_r = {n: Rot(nc, ph, "t_" + n, [128, 8, TT], BF16, 2) for n in ldn}
        xh_r = Rot(nc, ph, "t_xh", [128, NS, D], F32, 2)
        mg_r = Rot(nc, ph, "t_mg", [128, 8, TT], BF16, 2)
        hn2_r = Rot(nc, ph, "t_hn2", [128, 8, TT], BF16, 1)
        q2_r = Rot(nc, ph, "t_q2", [128, 8, TT], BF16, 1)
        o2_r = Rot(nc, ph, "t_o2", [128, 8, TT], BF16, 1)
        hn3_r = Rot(nc, ph, "t_hn3", [128, 8, TT], BF16, 1)
        t1_r = Rot(nc, ph, "t_t1", [128, TT], F32, 2)
        t2_r = Rot(nc, ph, "t_t2", [128, TT], F32, 2)
        p_r = Rot(nc, ph, "t_p", [128, 4, NMEM], F32, 1)
        pn_r = Rot(nc, ph, "t_pn", [128, 4, NMEM], BF16, 1)
        pT_r = Rot(nc, ph, "t_pT", [128, 8, 128], BF16, 1)
        sm_r = Rot(nc, ph, "t_sm", [128, 16], F32, 2)
        bc_reg = nc.gpsimd.to_reg(NSLOT - 1)
        mi = ph.enter_context(nc.sbuf_tensor("t_mi", [128, 128, 3], F32))
        mit = Tok()
        K.op("dve", lambda e: e.memset(mi[:, :, 0:1], float(S)), [], [mit])
        K.op("dve", lambda e: e.memset(mi[:, :, 1:2], 0.0), [], [mit])
        K.op("dve", lambda e: e.memset(mi[:, :, 2:3], float(2 * S)), [], [mit])
        meta_init = Tok(nowaw=True)
        K.dma("sp", meta_s.rearrange("(p j) c -> p j c", p=128), mi[:], K.slot("minit"), [mit], [DT["meta"], meta_init])

        def load_tile(T):
            tc_ = slice(T * TT, (T + 1) * TT)
            bufs = {}
            for n in ldn:
                buf, _ = ld_r[n].get()
                bufs[n] = buf
            i = ld_r["yaT"].i
            prev = ld_r["yaT"].toks[i]
            for n in ldn:
                K.dma("sp", bufs[n][:], lsrc[n][:, tc_].rearrange("(k p) t -> p k t", p=128), K.slot("tld%d" % i), [DT[n]], [prev])
            xh, xht = xh_r.get()
            K.dma("sp", xh[:], x_d[tc_, :].rearrange("(s p) d -> p s d", p=128), K.slot("txh%d" % xh_r.i), [], [xht])
            return bufs, prev, xh, xht

        tiles = {}
        mgs = {}

        def genA(T):
            L, ldt, xh, xht = tiles[T]
            tc_ = slice(T * TT, (T + 1) * TT)
            mg, mgt = mg_r.get()
            for dc in range(8):
                c = slice(dc * 128, (dc + 1) * 128)
                psA, tA = PS.get()
                psB, tB = PS.get()
                for kc in range(8):
                    K.mm(psA[:, 0:TT], Wa[:, kc, c], L["yaT"][:, kc, :], kc == 0, kc == 7, [Wat, ldt], [tA])
                for kc in range(8):
                    K.mm(psB[:, 0:TT], Wb[:, kc, c], L["ybT"][:, kc, :], kc == 0, kc == 7, [Wbt, ldt], [tB])
                t1, t1t = t1_r.get()
                t2, t2t = t2_r.get()
                K.tt("dve", t1[:], psA[:, 0:TT], L["sgAT"][:, dc, :], ALU.mult, [tA, ldt], [t1t])
                K.tt("dve", t2[:], psB[:, 0:TT], L["sgBT"][:, dc, :], ALU.mult, [tB, ldt], [t2t])
                K.tt("pool", mg[:, dc, :], t1[:], t2[:], ALU.add, [t1t, t2t], [mgt])
                if dc % 2 == 1:
                    yield
            for sub in range(NS):
                for half in range(2):
                    ps, pst = PS.get()
                    for kc in range(8):
                        K.mm(ps[:], mg[:, kc, sub * 128:(sub + 1) * 128], Wo_[:, kc, half * 512:(half + 1) * 512], kc == 0, kc == 7,
                             [mgt, Wot], [pst])
                    K.tt("dve", xh[:, sub, half * 512:(half + 1) * 512], ps[:], xh[:, sub, half * 512:(half + 1) * 512], ALU.add,
                         [pst, xht], [xht])
            if h1_s is not None:
                K.dma("sp", h1_s[tc_, :].rearrange("(s p) d -> p s d", p=128), xh[:], K.slot("h1d"), [xht], [DT["h1"]])
            mgs[T] = (mg, mgt)
            yield

        def genB(T):
            L, ldt, xh, xht = tiles[T]
            tc_ = slice(T * TT, (T + 1) * TT)
            hn2, hn2t = hn2_r.get()
            for sub in range(NS):
                norm_tile(xh[:, sub, :], xht, wBx[:], wBxt, hn2[:, :, sub * 128:(sub + 1) * 128], hn2t, tmp)
            q2, q2t = q2_r.get()
            for dc in range(8):
                ps, pst = PS.get()
                for kc in range(8):
                    K.mm(ps[:, 0:TT], Wq[:, kc, dc * 128:(dc + 1) * 128], hn2[:, kc, :], kc == 0, kc == 7, [Wqt, hn2t], [pst])
                K.cp("act", q2[:, dc, :], ps[:, 0:TT], [pst], [q2t])
                if dc % 4 == 3:
                    yield
            o2, o2t = o2_r.get()
            for sub in range(NS):
                sc_ = slice(sub * 128, (sub + 1) * 128)
                pss = [PS.get(), PS.get()]
                for hd in range(4):
                    psx, tx = pss[hd // 2]
                    for j in range(2):
                        K.mm(psx[:, (hd % 2) * 256:(hd % 2) * 256 + 256], q2[:, 2 * hd + j, sc_], KT[:, 2 * hd + j, :], j == 0, j == 1,
                             [q2t, KTt], [tx])
                sm, smt = sm_r.get()
                for i2 in range(2):
                    psx, tx = pss[i2]
                    K.op("dve", lambda e: e.tensor_reduce(out=sm[:, 2 * i2:2 * i2 + 2], in_=psx[:].rearrange("p (h m) -> p h m", h=2),
                                                          axis=AX.X, op=ALU.max), [tx], [smt])
                K.ts("dve", sm[:, 4:8], sm[:, 0:4], -1.0 / 16.0, None, ALU.mult, None, [smt], [smt])
                p, pt = p_r.get()
                for hd in range(4):
                    psx, tx = pss[hd // 2]
                    K.act(p[:, hd, :], psx[:, (hd % 2) * 256:(hd % 2) * 256 + 256], AF.Exp, [tx, smt], [pt, smt],
                          scale=1.0 / 16.0, bias=sm[:, 4 + hd:5 + hd], accum_out=sm[:, 8 + hd:9 + hd])
                K.op("dve", lambda e: e.reciprocal(out=sm[:, 12:16], in_=sm[:, 8:12]), [smt], [smt])
                pn, pnt = pn_r.get()
                K.tt("pool", pn[:], p[:], sm[:, 12:16].unsqueeze(2).to_broadcast([128, 4, NMEM]), ALU.mult, [pt, smt], [pnt])
                yield
                ps, pst = PS.get()
                psb = ps[:].bitcast(BF16)
                for hd in range(4):
                    for mc in range(2):
                        K.tr(psb[:, (hd * 2 + mc) * 128:(hd * 2 + mc + 1) * 128], pn[:, hd, mc * 128:(mc + 1) * 128], C.identb[:],
                             [pnt, ctok], [pst])
                pT, pTt = pT_r.get()
                K.cp("act", pT[:], psb.rearrange("p (a t) -> p a t", a=8), [pst], [pTt])
                yield
                for og in range(2):
                    pso, tso = PS.get()
                    for d4 in range(4):
                        dc = og * 4 + d4
                        hd = dc // 2
                        for mc in range(2):
                            K.mm(pso[:, d4 * 128:(d4 + 1) * 128], V[:, mc, dc * 128:(dc + 1) * 128], pT[:, hd * 2 + mc, :], mc == 0, mc == 1,
                                 [Vt, pTt], [tso])
                    K.cp("act", o2[:, og * 4:og * 4 + 4, sc_], pso[:].rearrange("p (a t) -> p a t", a=4), [tso], [o2t])
                    yield
            for sub in range(NS):
                for half in range(2):
                    ps, pst = PS.get()
                    for kc in range(8):
                        K.mm(ps[:], o2[:, kc, sub * 128:(sub + 1) * 128], Wxo[:, kc, half * 512:(half + 1) * 512], kc == 0, kc == 7,
                             [o2t, Wxot], [pst])
                    K.tt("dve", xh[:, sub, half * 512:(half + 1) * 512], ps[:], xh[:, sub, half * 512:(half + 1) * 512], ALU.add,
                         [pst, xht], [xht])
            K.dma("sp", h2_s[tc_, :].rearrange("(s p) d -> p s d", p=128), xh[:], K.slot("h2d%d" % xh_r.i), [xht], [DT["h2"]])
            yield
            hn3, hn3t = hn3_r.get()
            for sub in range(NS):
                xs_, xst_ = norm_tile(xh[:, sub, :], xht, wBf[:], wBft, hn3[:, :, sub * 128:(sub + 1) * 128], hn3t, tmp)
                r0 = T * TT + sub * 128
                K.dma("sp", hn3k_s[r0:r0 + 128, :], xs_[:], K.slot("hn3k%d" % tmp["xs"].i), [xst_], [DT["hn3k"]])
            if hn3T_s is not None:
                K.dma("sp", hn3T_s[:, tc_].rearrange("(k p) t -> p k t", p=128), hn3[:], K.slot("hn3d%d" % hn3_r.i), [hn3t], [DT["hn3T"]])
            for sub in range(NS):
                ps, pst = PS.get()
                for kc in range(8):
                    K.mm(ps[:, 0:36], hn3[:, kc, sub * 128:(sub + 1) * 128], Wr[:, kc, :], kc == 0, kc == 7, [hn3t, Wrt], [pst])
                si_ = T * NS + sub
                K.tt("dve", lgall[:, si_, :], ps[:, 0:36], brB[:], ALU.add, [pst, brt], [lgt])
            yield

        NTL = S // TT
        tiles[0] = load_tile(0)
        if NTL > 1:
            tiles[1] = load_tile(1)
        for _ in genA(0):
            pass
        for T in range(NTL):
            gens = [genB(T)]
            if T + 1 < NTL:
                gens.append(genA(T + 1))
            run_lockstep2(gens)
            del tiles[T]
            if T + 2 < NTL:
                tiles[T + 2] = load_tile(T + 2)
        K.barrier()
    wst.close()
    with ExitStack() as pr:
        A3 = lambda name, dt_=F32: pr.enter_context(nc.sbuf_tensor(name, [128, NT, NE], dt_))
        A2 = lambda name, dt_=F32: pr.enter_context(nc.sbuf_tensor(name, [128, NT], dt_))
        el, ex, m_, cw, rk, cs, pa, pb = (A3("r_%s" % n) for n in ("el", "ex", "m", "cw", "rk", "cs", "pa", "pb"))
        mb = A3("r_mb", BF16)
        g4 = pr.enter_context(nc.sbuf_tensor("r_g4", [128, NT, 4], F32))
        oh = pr.enter_context(nc.sbuf_tensor("r_oh", [128, NT, 4], F32))
        gmx, gs, l1, l2, den, k1, k2, w1, w2, t2a = (A2("r2_%s" % n) for n in ("gmx", "gs", "l1", "l2", "den", "k1", "k2", "w1", "w2", "t2a"))
        scin = pr.enter_context(nc.sbuf_tensor("r_scin", [128, NT, 2, 3], F32))
        posf = pr.enter_context(nc.sbuf_tensor("r_posf", [128, NT, 2], F32))
        posi = pr.enter_context(nc.sbuf_tensor("r_posi", [128, NT, 2], I32))
        rt = Tok()
        B3 = lambda a2: a2[:].unsqueeze(2).to_broadcast([128, NT, NE])
        Gv = lgall[:, :, 0:4]
        Ev = lgall[:, :, 4:36]
        RD = lambda out, in_, op: K.op("dve", lambda e: e.tensor_reduce(out=out, in_=in_, axis=AX.X, op=op), [rt, lgt], [rt])
        TTd = lambda out, a, b, op: K.tt("dve", out, a, b, op, [rt, lgt, ctok], [rt])
        RD(gmx[:], Gv, ALU.max)
        TTd(g4[:], Gv, gmx[:].unsqueeze(2).to_broadcast([128, NT, 4]), ALU.subtract)
        TTd(oh[:], Gv, gmx[:].unsqueeze(2).to_broadcast([128, NT, 4]), ALU.is_ge)
        K.act(g4[:], g4[:], AF.Exp, [rt], [rt])
        RD(gs[:], g4[:], ALU.add)
        K.op("dve", lambda e: e.reciprocal(out=gs[:], in_=gs[:]), [rt], [rt])
        K.ts("dve", oh[:], oh[:], -1.0, 1.0e9, ALU.add, ALU.mult, [rt], [rt])
        TTd(el[:].rearrange("p s (g e) -> p s g e", g=4), Ev.rearrange("p s (g e) -> p s g e", g=4),
            oh[:].unsqueeze(3).to_broadcast([128, NT, 4, 8]), ALU.add)
        RD(l1[:], el[:], ALU.max)
        TTd(m_[:], el[:], B3(l1), ALU.is_ge)
        K.stt("dve", ex[:], m_[:], -1.0e9, el[:], ALU.mult, ALU.add, [rt], [rt])
        RD(l2[:], ex[:], ALU.max)
        TTd(ex[:], el[:], B3(l1), ALU.subtract)
        K.act(ex[:], ex[:], AF.Exp, [rt], [rt])
        TTd(m_[:], el[:], B3(l2), ALU.is_ge)
        TTd(cw[:], ex[:], m_[:], ALU.mult)
        RD(den[:], cw[:], ALU.add)
        K.op("dve", lambda e: e.reciprocal(out=den[:], in_=den[:]), [rt], [rt])
        TTd(den[:], den[:], gs[:], ALU.mult)
        TTd(cw[:], cw[:], B3(den), ALU.mult)
        if "comb" in dbg:
            K.dma("sp", comb_s.rearrange("(s p) e -> p s e", p=128), cw[:], K.slot("cbd"), [rt], [DT["comb"]])
        K.ts("dve", mb[:], cw[:], 0.0, None, ALU.is_gt, None, [rt], [rt])
        for half in range(2):
            psR, tR = PS.get()
            psC, tC = PS.get()
            for j in range(16):
                s_ = half * 16 + j
                K.mm(psR[:, j * NE:(j + 1) * NE], C.UT[:], mb[:, s_, :], True, True, [rt, ctok], [tR])
                K.mm(psC[:, j * NE:(j + 1) * NE], C.onesb[:], mb[:, s_, :], True, True, [rt, ctok], [tC])
            K.cp("dve", rk[:, half * 16:(half + 1) * 16, :], psR[:].rearrange("p (s e) -> p s e", e=NE), [tR], [rt])
            K.cp("dve", cs[:, half * 16:(half + 1) * 16, :], psC[:].rearrange("p (s e) -> p s e", e=NE), [tC], [rt])
        src = cs
        for k_, dst in zip((1, 2, 4, 8, 16), (pa, pb, pa, pb, pa)):
            K.cp("dve", dst[:, 0:k_, :], src[:, 0:k_, :], [rt], [rt])
            TTd(dst[:, k_:NT, :], src[:, k_:NT, :], src[:, 0:NT - k_, :], ALU.add)
            src = dst
        TTd(pb[:], src[:], cs[:], ALU.subtract)
        TTd(rk[:], rk[:], pb[:], ALU.add)
        TTd(pa[:], rk[:], C.ecap[:].unsqueeze(1).to_broadcast([128, NT, NE]), ALU.add)
        K.ts("dve", pb[:], rk[:], float(CAP), None, ALU.is_lt, None, [rt], [rt])
        TTd(pb[:], pb[:], mb[:], ALU.mult)
        K.ts("dve", pa[:], pa[:], -1.0, BIG, ALU.mult, ALU.add, [rt], [rt])
        TTd(pa[:], pa[:], pb[:], ALU.mult)
        RD(k1[:], pa[:], ALU.max)
        TTd(m_[:], pa[:], B3(k1), ALU.is_ge)
        K.stt("dve", ex[:], m_[:], -2.0 * BIG, pa[:], ALU.mult, ALU.add, [rt], [rt])
        RD(k2[:], ex[:], ALU.max)
        K.ts("dve", k2[:], k2[:], 0.0, None, ALU.max, None, [rt], [rt])
        for kk, wk, col in ((k1, w1, 0), (k2, w2, 1)):
            TTd(m_[:], pa[:], B3(kk), ALU.is_equal)
            TTd(m_[:], m_[:], cw[:], ALU.mult)
            RD(wk[:], m_[:], ALU.add)
            K.cp("dve", scin[:, :, col, 1], wk[:], [rt], [rt])
            K.cp("dve", scin[:, :, col, 0], C.tokall[:], [rt, ctok], [rt])
            K.ts("dve", scin[:, :, col, 2], C.tokall[:], float(col * S), None, ALU.add, None, [rt, ctok], [rt])
            K.ts("dve", posf[:, :, col], kk[:], -1.0, BIG, ALU.mult, ALU.add, [rt], [rt])
        K.cp("dve", posi[:], posf[:], [rt], [rt])
        for s_ in range(NT):
            for k2_ in range(2):
                K.idma(meta_s[:, :], bass.IndirectOffsetOnAxis(ap=posi[:, s_, k2_:k2_ + 1], axis=0), scin[:, s_, k2_, :], None,
                       K.slot("msc%d" % ((2 * s_ + k2_) % 8)), [rt, meta_init], [DT["meta"]],
                       bounds_check=bc_reg, oob_is_err=False)
        K.barrier()
    if stop == "P8":
        raise _Stop()

    with ExitStack() as ph:
        wBn = ph.enter_context(nc.sbuf_tensor("wBn", [128, D], F32))
        wBnt = Tok()
        K.dma("sp", wBn[:], nw_d["nw_fin"].partition_broadcast(128), K.slot("wB"), [], [wBnt])
        NTE = CAP // 128
        with ExitStack() as pe_:
            wg_r = Rot(nc, pe_, "e_wg", [128, 8, DFF], BF16, 4)
            wu_r = Rot(nc, pe_, "e_wu", [128, 8, DFF], BF16, 4)
            wd_r = Rot(nc, pe_, "e_wd", [128, 4, D], BF16, 4)
            mt_r = Rot(nc, pe_, "e_mt", [128, NTE, 3], F32, 4)
            ix_r = Rot(nc, pe_, "e_ix", [128, 1], I32, 4 * NTE)
            xg_r = Rot(nc, pe_, "e_xg", [128, D], BF16, 12)
            XT_r = Rot(nc, pe_, "e_XT", [128, 8, CAP], BF16, 3)
            hT_r = Rot(nc, pe_, "e_hT", [128, 4, CAP], BF16, 3)
            sg_r = Rot(nc, pe_, "e_sg", [128, CAP], F32, 2)
            ys_r = Rot(nc, pe_, "e_ys", [128, D], BF16, 4)
            zr2 = pe_.enter_context(nc.sbuf_tensor("e_zr", [128, D], BF16))
            zr2t = Tok()
            ytok_init = Tok(nowaw=True)
            K.op("dve", lambda e: e.memset(zr2[:], 0.0), [], [zr2t])
            K.dma("sp", hn3k_s[S:S + 1, :], zr2[0:1, :], K.slot("zini"), [zr2t], [DT["hn3k"]])
            for j_ in range(2 * S // 1024):
                K.dma("sp", ytok_s[j_ * 1024:(j_ + 1) * 1024, :].rearrange("(a p) d -> p a d", p=128),
                      zr2[:].unsqueeze(1).to_broadcast([128, 8, D]), K.slot("zini"), [zr2t], [DT["ytok"], ytok_init])
            dx_r = Rot(nc, pe_, "e_dx", [128, 1], I32, 8)
            bc2_reg = nc.gpsimd.to_reg(2 * S - 1)
            def expert_gen(e_):
                wg, wgt = wg_r.get()
                wu, wut = wu_r.get()
                wd, wdt = wd_r.get()
                i = wg_r.i
                K.dma("pool", wg[:], wg_d[e_].rearrange("(kc p) f -> p kc f", p=128), K.slot("ewg%d" % i), [], [wgt])
                K.dma("pool", wu[:], wu_d[e_].rearrange("(kc p) f -> p kc f", p=128), K.slot("ewu%d" % i), [], [wut])
                K.dma("pool", wd[:], wd_d[e_].rearrange("(fc p) d -> p fc d", p=128), K.slot("ewd%d" % i), [], [wdt])
                mt, mtt = mt_r.get()
                K.dma("sp", mt[:], meta_s[e_ * CAP:(e_ + 1) * CAP, :].rearrange("(t p) c -> p t c", p=128), K.slot("emt%d" % mt_r.i),
                      [DT["meta"]], [mtt])
                xgs = []
                for t in range(NTE):
                    ix, ixt = ix_r.get()
                    K.cp("dve", ix[:], mt[:, t, 0:1], [mtt], [ixt])
                    xg, xgt = xg_r.get()
                    K.idma(xg[:, :], None, hn3k_s[:, :], bass.IndirectOffsetOnAxis(ap=ix[:, :], axis=0),
                           K.slot("exg%d" % xg_r.i), [ixt, DT["hn3k"]], [xgt])
                    xgs.append((xg, xgt))
                yield
                XT, XTt = XT_r.get()
                for t in range(NTE):
                    xg, xgt = xgs[t]
                    ps, pst = PS.get()
                    psb = ps[:].bitcast(BF16)
                    for kc in range(8):
                        K.tr(psb[:, kc * 128:(kc + 1) * 128], xg[:, kc * 128:(kc + 1) * 128], C.identb[:], [xgt, ctok], [pst])
                    K.cp("act", XT[:, :, t * 128:(t + 1) * 128], psb.rearrange("p (k t) -> p k t", k=8), [pst], [XTt])
                yield
                hT, hTt = hT_r.get()
                for fc in range(4):
                    c = slice(fc * 128, (fc + 1) * 128)
                    psG, tG = PS.get()
                    psU, tU = PS.get()
                    for kc in range(8):
                        K.mm(psG[:, 0:CAP], wg[:, kc, c], XT[:, kc, :], kc == 0, kc == 7, [wgt, XTt], [tG])
                    for kc in range(8):
                        K.mm(psU[:, 0:CAP], wu[:, kc, c], XT[:, kc, :], kc == 0, kc == 7, [wut, XTt], [tU])
                    sg, sgt = sg_r.get()
                    K.act(sg[:], psG[:, 0:CAP], AF.Silu, [tG], [sgt])
                    K.tt("dve", hT[:, fc, :], sg[:], psU[:, 0:CAP], ALU.mult, [sgt, tU], [hTt])
                yield
                for t in range(NTE):
                    ys, yst = ys_r.get()
                    for half in range(2):
                        psY, tY = PS.get()
                        for fc in range(4):
                            K.mm(psY[:], hT[:, fc, t * 128:(t + 1) * 128], wd[:, fc, half * 512:(half + 1) * 512], fc == 0, fc == 3,
                                 [hTt, wdt], [tY])
                        if half == 0:
                            K.act(ys[:, 0:512], psY[:], AF.Copy, [tY, mtt], [yst], scale=mt[:, t, 1:2])
                        else:
                            K.ts("dve", ys[:, 512:1024], psY[:], mt[:, t, 1:2], None, ALU.mult, None, [tY, mtt], [yst])
                    dx, dxt = dx_r.get()
                    K.cp("dve", dx[:], mt[:, t, 2:3], [mtt], [dxt])
                    K.idma(ytok_s[:, :], bass.IndirectOffsetOnAxis(ap=dx[:, :], axis=0), ys[:, :], None,
                           K.slot("eys%d" % ys_r.i), [yst, dxt, ytok_init], [DT["ytok"]],
                           bounds_check=bc2_reg, oob_is_err=False)
                yield

            run_skewed((expert_gen(e_) for e_ in range(NE)), 3)
            K.barrier()
        acc_r = Rot(nc, ph, "f_acc", [128, D], F32, 8)
        y1_r = Rot(nc, ph, "f_y1", [128, D], BF16, 8)
        y2_r = Rot(nc, ph, "f_y2", [128, D], BF16, 8)
        st_r = Rot(nc, ph, "f_st", [128, 4], F32, 4)
        jk_r = Rot(nc, ph, "f_jk", [128, D], BF16, 2)
        ot_r = Rot(nc, ph, "f_ot", [128, D], F32, 4)
        def fload(t):
            r0 = t * 128
            acc, acct = acc_r.get()
            i = acc_r.i
            K.dma("sp", acc[:], h2_s[r0:r0 + 128, :], K.slot("facc%d" % i), [DT["h2"]], [acct])
            y1, y1t = y1_r.get()
            y2, y2t = y2_r.get()
            K.dma("sp", y1[:], ytok_s[r0:r0 + 128, :], K.slot("fy1%d" % i), [DT["ytok"]], [y1t])
            K.dma("sp", y2[:], ytok_s[S + r0:S + r0 + 128, :], K.slot("fy2%d" % i), [DT["ytok"]], [y2t])
            return acc, acct, y1, y1t, y2, y2t

        LA = 7
        pend = [fload(t) for t in range(min(LA, NT))]
        for t in range(NT):
            r0 = t * 128
            acc, acct, y1, y1t, y2, y2t = pend.pop(0)
            K.tt("dve", acc[:], acc[:], y1[:], ALU.add, [acct, y1t], [acct])
            K.tt("pool", acc[:], acc[:], y2[:], ALU.add, [acct, y2t], [acct])
            jk, jkt = jk_r.get()
            st, stt_ = st_r.get()
            K.act(jk[:], acc[:], AF.Square, [acct], [jkt, stt_], accum_out=st[:, 0:1])
            K.act(st[:, 1:2], st[:, 0:1], AF.Sqrt, [stt_, ctok], [stt_], scale=1.0 / D, bias=C.epsc[:, 0:1])
            K.op("dve", lambda e: e.reciprocal(out=st[:, 2:3], in_=st[:, 1:2]), [stt_], [stt_])
            ot, ott = ot_r.get()
            K.stt("dve", ot[:], acc[:], st[:, 2:3], wBn[:], ALU.mult, ALU.mult, [acct, stt_, wBnt], [ott])
            K.dma("sp", out_d[r0:r0 + 128, :], ot[:], K.slot("eout%d" % ot_r.i), [ott], [])
            if t + LA < NT:
                pend.append(fload(t + LA))
        K.barrier()


def make_in_maps(inputs):
    hc = host_consts()
    f = lambda a: np.ascontiguousarray(np.asarray(a, dtype=np.float32))
    shared = {
        "w_in": f(inputs["w_in"][0]),
        "cw": f(np.asarray(inputs["conv_w"][0]).T.reshape(24, 128, 4).transpose(1, 0, 2)),
        "a_log": f(inputs["gdn_a_log"][0]).reshape(8, 1),
        "dt_bias": f(inputs["gdn_dt_bias"][0]).reshape(8, 1),
        "gdn_nw": f(inputs["gdn_out_norm_w"][0]).reshape(1, 128),
        "hgrn_nw": f(inputs["hgrn_out_norm_w"][0]).reshape(1, 128),
        "lbT": f(np.asarray(inputs["hgrn_lb"]).reshape(2, 8, 128).transpose(2, 0, 1)),
        "w_a": f(inputs["w_branch_a"][0]), "w_b": f(inputs["w_branch_b"][0]), "w_out": f(inputs["w_out"][0]),
        "wq": f(inputs["xattn_wq"][0]), "wkv": f(inputs["xattn_wkv"][0]), "wo": f(inputs["xattn_wo"][0]),
        "nw_mix": f(inputs["norm_mix_w"][0]).reshape(1, D), "nw_xa": f(inputs["norm_xattn_w"][0]).reshape(1, D),
        "nw_mem": f(inputs["norm_mem_w"][0]).reshape(1, D), "nw_ffn": f(inputs["norm_ffn_w"][0]).reshape(1, D),
        "nw_fin": f(inputs["final_norm_w"]).reshape(1, D),
        "wr": f(np.concatenate([np.asarray(inputs["router_group_w"][0]), np.asarray(inputs["router_expert_w"][0])], axis=1)),
        "br": f(np.concatenate([np.asarray(inputs["router_group_b"][0]), np.asarray(inputs["router_expert_b"][0])])).reshape(1, 36),
        "wg": f(inputs["expert_w_gate"][0]), "wu": f(inputs["expert_w_up"][0]), "wd": f(inputs["expert_w_down"][0]),
    }
    for k, v in hc.items():
        shared["c_" + k] = np.ascontiguousarray(v)
    shared["c_rmask"] = host_rmask()
    maps = []
    for b in range(8):
        m = dict(shared)
        m["x"] = f(inputs["x"][b])
        m["mem"] = f(inputs["mem"][b])
        maps.append(m)
    return maps


def kernel(**inputs):
    nc = build()
    in_maps = make_in_maps(inputs)
    res = run_bass_kernel_spmd(nc, in_maps, core_ids=list(range(8)))
    return np.stack([np.asarray(r["out"], dtype=np.float32) for r in res.results], axis=0)
```

```python
import os
import numpy as np
import ml_dtypes
from contextlib import ExitStack
import concourse.bass as bass
import concourse.mybir as mybir
from concourse.bass_utils import run_bass_kernel_spmd

F32 = mybir.dt.float32
F32R = mybir.dt.float32r
BF16 = mybir.dt.bfloat16
AF = mybir.ActivationFunctionType
ALU = mybir.AluOpType
AX = mybir.AxisListType

S = 4096
D = 1024
NT = S // 128
EPS = 1e-6
NE = 32
DFF = 512
NMEM = 256
INC = 10256
CAP = 512
NSLOT = NE * CAP
BIG = 65536.0
I32 = mybir.dt.int32


class Tok:
    __slots__ = ("w", "r", "nowaw")

    def __init__(self, nowaw=False):
        self.w = {}
        self.r = {}
        self.nowaw = nowaw


class Slot:
    def __init__(self, key, sem):
        self.key = key
        self.sem = sem
        self.cum = 0


class KB:
    def __init__(self, nc, es):
        self.nc = nc
        self.es = es
        self.E = {"pe": nc.tensor, "act": nc.scalar, "dve": nc.vector, "pool": nc.gpsimd, "sp": nc.sync}
        self.semh = {}
        self.cnt = {}
        self.known = {e: {} for e in self.E}
        for e in ("pe", "act", "dve", "pool"):
            self.semh[e] = es.enter_context(nc.semaphore("s_" + e))
            self.cnt[e] = 0
        self.slots = {}
        self.free_slots = []
        self.free_slots_sw = []
        self.slot_by_key = {}
        self.n_inst = 0

    SW_PREFIXES = ("wblk", "w_", "ewg", "ewu", "ewd", "exg", "eys", "msc")

    def slot(self, name):
        if name not in self.slots:
            sw = name.startswith(self.SW_PREFIXES)
            pool = self.free_slots_sw if sw else self.free_slots
            if pool:
                self.slots[name] = pool.pop()
            else:
                key = "d_%d" % len(self.slot_by_key)
                sem = self.es.enter_context(self.nc.semaphore(key))
                self.semh[key] = sem
                sl = Slot(key, sem)
                sl.sw = sw
                self.slot_by_key[key] = sl
                self.slots[name] = sl
        return self.slots[name]

    def _emit_waits(self, e, reads, writes, skip_w_key=None):
        need = {}
        for t in reads:
            for k, v in t.w.items():
                if need.get(k, 0) < v:
                    need[k] = v
        for t in writes:
            if t.nowaw:
                continue
            for k, v in t.w.items():
                if k == skip_w_key:
                    continue
                if need.get(k, 0) < v:
                    need[k] = v
            for k, v in t.r.items():
                if need.get(k, 0) < v:
                    need[k] = v
        kn = self.known[e]
        for k, v in need.items():
            if e == "pe" and k == "pe":
                continue
            if kn.get(k, 0) >= v:
                continue
            self.E[e].wait_ge(self.semh[k], v)
            kn[k] = v

    def op(self, e, fn, reads=(), writes=()):
        self._emit_waits(e, reads, writes)
        inst = fn(self.E[e])
        self.cnt[e] += 1
        c = self.cnt[e]
        inst.then_inc(self.semh[e], 1)
        for t in reads:
            t.r[e] = c
        for t in writes:
            t.w[e] = c
        self.n_inst += 1
        return inst

    def dma(self, q, out, in_, slot, reads=(), writes=(), **kw):
        assert getattr(slot, "sw", False) == (q == "pool"), (q, slot.key)
        self._emit_waits(q, reads, writes, skip_w_key=slot.key)
        inst = self.E[q].dma_start(out=out, in_=in_, **kw)
        inst.then_inc(slot.sem, 16)
        slot.cum += 16
        for t in reads:
            t.r[slot.key] = slot.cum
        for t in writes:
            t.w[slot.key] = slot.cum
        self.n_inst += 1
        return inst

    def idma(self, out, out_offset, in_, in_offset, slot, reads=(), writes=(), **kw):
        assert getattr(slot, "sw", False), slot.key
        self._emit_waits("pool", reads, writes, skip_w_key=slot.key)
        inst = self.E["pool"].indirect_dma_start(out=out, out_offset=out_offset, in_=in_, in_offset=in_offset, **kw)
        inst.then_inc(slot.sem, 16)
        slot.cum += 16
        for t in reads:
            t.r[slot.key] = slot.cum
        for t in writes:
            t.w[slot.key] = slot.cum
        self.n_inst += 1
        return inst

    def barrier(self):
        for e in self.E:
            kn = self.known[e]
            for k, sem in self.semh.items():
                v = self.cnt[k] if k in self.cnt else self.slot_by_key[k].cum
                if v == 0 or kn.get(k, 0) >= v:
                    continue
                self.E[e].wait_ge(sem, v)
                kn[k] = v
        for sl in self.slots.values():
            (self.free_slots_sw if getattr(sl, "sw", False) else self.free_slots).append(sl)
        self.slots = {}

    def mm(self, out, lhsT, rhs, start, stop, reads, writes):
        return self.op("pe", lambda e: e.matmul(out, lhsT, rhs, start=start, stop=stop), reads, writes)

    def tr(self, out, in_, ident, reads, writes):
        return self.op("pe", lambda e: e.transpose(out, in_, ident), reads, writes)

    def act(self, out, in_, func, reads, writes, **kw):
        return self.op("act", lambda e: e.activation(out=out, in_=in_, func=func, **kw), reads, writes)

    def tt(self, eng, out, in0, in1, op, reads, writes):
        return self.op(eng, lambda e: e.tensor_tensor(out=out, in0=in0, in1=in1, op=op), reads, writes)

    def ts(self, eng, out, in0, s1, s2, op0, op1, reads, writes):
        if op1 is None:
            return self.op(eng, lambda e: e.tensor_scalar(out=out, in0=in0, scalar1=s1, scalar2=None, op0=op0), reads, writes)
        return self.op(eng, lambda e: e.tensor_scalar(out=out, in0=in0, scalar1=s1, scalar2=s2, op0=op0, op1=op1), reads, writes)

    def stt(self, eng, out, in0, scalar, in1, op0, op1, reads, writes):
        return self.op(eng, lambda e: e.scalar_tensor_tensor(out=out, in0=in0, scalar=scalar, in1=in1, op0=op0, op1=op1), reads, writes)

    def cp(self, eng, out, in_, reads, writes):
        if eng == "act":
            return self.act(out, in_, AF.Copy, reads, writes)
        return self.op(eng, lambda e: e.tensor_copy(out=out, in_=in_), reads, writes)


def run_lockstep2(gens):
    gens = list(gens)
    while gens:
        for gg in list(gens):
            try:
                next(gg)
            except StopIteration:
                gens.remove(gg)


def run_skewed(gen_iter, width):
    it = iter(gen_iter)
    active = []
    exhausted = False
    while True:
        if not exhausted and len(active) < width:
            try:
                active.append(next(it))
            except StopIteration:
                exhausted = True
        if not active:
            if exhausted:
                break
            continue
        for g in list(active):
            try:
                next(g)
            except StopIteration:
                active.remove(g)


class PsumPool:
    def __init__(self, K, nc, es, n=8):
        self.banks = [es.enter_context(nc.psum_tensor("psb%d" % i, [128, 512], F32)) for i in range(n)]
        self.toks = [Tok() for _ in range(n)]
        self.i = 0
        self.n = n
        self.held = set()

    def get(self, hold=False):
        for _ in range(self.n):
            j = self.i
            self.i = (self.i + 1) % self.n
            if j not in self.held:
                if hold:
                    self.held.add(j)
                return self.banks[j], self.toks[j]
        raise RuntimeError("all PSUM banks held")

    def rel(self, bank):
        for j, b in enumerate(self.banks):
            if b is bank:
                self.held.discard(j)
                return


class Rot:
    def __init__(self, nc, es, name, shape, dtype, n):
        self.bufs = [es.enter_context(nc.sbuf_tensor("%s%d" % (name, i), shape, dtype)) for i in range(n)]
        self.toks = [Tok() for _ in range(n)]
        self.i = 0
        self.n = n

    def get(self):
        b, t = self.bufs[self.i], self.toks[self.i]
        self.i = (self.i + 1) % self.n
        return b, t


def host_rmask():
    t = np.arange(S)
    return np.broadcast_to(((t % 64) != 0).astype(np.float32)[None, :], (128, S)).copy()


def host_consts():
    c = {}
    c["identf"] = np.eye(128, dtype=np.float32)
    c["identb"] = np.eye(128, dtype=np.float32).astype(ml_dtypes.bfloat16)
    c["onesb"] = np.ones((128, 128), np.float32).astype(ml_dtypes.bfloat16)
    sel = np.zeros((8, 8, 128), np.float32)
    for h in range(8):
        sel[h, h, :] = 1.0
    c["selh"] = sel
    c["nselh"] = -sel
    i = np.arange(128)[:, None]
    j = np.arange(128)[None, :]
    same = (i // 64) == (j // 64)
    NEG = -30000.0
    c["mnegS"] = np.where(same & (i > j), 0.0, -NEG).astype(np.float32)
    c["mnegIT"] = np.where(same & (j >= i), 0.0, NEG).astype(np.float32)
    jj = (np.arange(128) % 64)[:, None]
    ii = np.arange(64)[None, :]
    c["maskT64"] = (ii >= jj).astype(np.float32)
    c["epsc"] = np.full((128, 1), EPS, np.float32)
    c["UT"] = (np.arange(128)[:, None] < np.arange(128)[None, :]).astype(np.float32).astype(ml_dtypes.bfloat16)
    c["ecap"] = np.broadcast_to((np.arange(NE) * CAP).astype(np.float32)[None, :], (128, NE)).copy()
    c["tokb"] = np.arange(128, dtype=np.float32).reshape(128, 1)
    c["tokall"] = (np.arange(128)[:, None] + 128 * np.arange(NT)[None, :]).astype(np.float32)
    c["onec"] = np.ones((128, 1), np.float32)
    return c


CONST_DT = {"identb": BF16, "onesb": BF16, "UT": BF16}


class Cn:
    pass


class _Stop(Exception):
    pass


def build(dbg=(), stop=None):
    nc = bass.Bass("TRN2", target_bir_lowering=False)
    es = ExitStack()
    K = KB(nc, es)
    try:
        _build(nc, es, K, dbg, stop)
    except _Stop:
        K.barrier()
        return nc
    K.barrier()
    es.close()
    return nc


def _build(nc, es, K, dbg, stop):

    def ext_in(name, shape, dt=F32):
        return nc.dram_tensor(name, list(shape), dt, kind="ExternalInput").ap()

    def scratch(name, shape, dt):
        kind = "ExternalOutput" if name in dbg else "Internal"
        return nc.dram_tensor(name, list(shape), dt, kind=kind).ap()

    x_d = ext_in("x", [S, D])
    mem_d = ext_in("mem", [NMEM, D])
    w_in_d = ext_in("w_in", [D, INC])
    cw_d = ext_in("cw", [128, 24, 4])
    alog_d = ext_in("a_log", [8, 1])
    dtb_d = ext_in("dt_bias", [8, 1])
    gnw_d = ext_in("gdn_nw", [1, 128])
    hnw_d = ext_in("hgrn_nw", [1, 128])
    lb_d = ext_in("lbT", [128, 2, 8])
    wa_d = ext_in("w_a", [D, D])
    wb_d = ext_in("w_b", [D, D])
    wout_d = ext_in("w_out", [D, D])
    wq_d = ext_in("wq", [D, D])
    wkv_d = ext_in("wkv", [D, 2 * D])
    wo_d = ext_in("wo", [D, D])
    nw_d = {n: ext_in(n, [1, D]) for n in ("nw_mix", "nw_xa", "nw_mem", "nw_ffn", "nw_fin")}
    wr_d = ext_in("wr", [D, 36])
    br_d = ext_in("br", [1, 36])
    wg_d = ext_in("wg", [NE, D, DFF])
    wu_d = ext_in("wu", [NE, D, DFF])
    wd_d = ext_in("wd", [NE, DFF, D])
    hc = host_consts()
    cdram = {k: ext_in("c_" + k, v.shape, CONST_DT.get(k, F32)) for k, v in hc.items()}
    rmask_d = ext_in("c_rmask", [128, S])
    out_d = nc.dram_tensor("out", [S, D], F32, kind="ExternalOutput").ap()

    qkvT_s = scratch("qkvT", [3072, S], BF16)
    abT_s = scratch("abT", [16, S], F32)
    ogA_s = scratch("ogA", [S, D], BF16)
    fT_s = scratch("fT", [D, S], BF16)
    qbT_s = scratch("qbT", [D, S], BF16)
    ib_s = scratch("ib", [S, D], BF16)
    ogB_s = scratch("ogB", [S, D], BF16)
    sgAT_s = scratch("sgAT", [D, S], BF16)
    sgBT_s = scratch("sgBT", [D, S], BF16)
    DT = {}
    for n in ("qkvT", "abT", "ogA", "fT", "qbT", "ib", "ogB", "sgAT", "sgBT"):
        DT[n] = Tok(nowaw=True)

    PS = PsumPool(K, nc, es)
    C = Cn()
    ctok = Tok()
    cs = K.slot("const")
    for k, v in hc.items():
        t = es.enter_context(nc.sbuf_tensor("k_" + k, list(v.shape), CONST_DT.get(k, F32)))
        setattr(C, k, t)
        K.dma("sp", t[:], cdram[k], cs, [], [ctok])

    def norm_tile(xt, xtok, wB, wBtok, dst, dsttok, tmp):
        junk, jt = tmp["junk"].get()
        st, stok = tmp["st"].get()
        xs, xst = tmp["xs"].get()
        K.act(junk[:], xt, AF.Square, [xtok], [jt, stok], accum_out=st[:, 0:1])
        K.act(st[:, 1:2], st[:, 0:1], AF.Sqrt, [stok, ctok], [stok], scale=1.0 / D, bias=C.epsc[:, 0:1])
        K.op("dve", lambda e: e.reciprocal(out=st[:, 2:3], in_=st[:, 1:2]), [stok], [stok])
        K.stt("dve", xs[:], xt, st[:, 2:3], wB, ALU.mult, ALU.mult, [xtok, stok, wBtok], [xst])
        ps, pst = PS.get()
        psb = ps[:].bitcast(BF16)
        for kc in range(8):
            K.tr(psb[:, kc * 128:(kc + 1) * 128], xs[:, kc * 128:(kc + 1) * 128], C.identb[:], [xst, ctok], [pst])
        K.cp("act", dst, psb.rearrange("p (k t) -> p k t", k=8), [pst], [dsttok])
        return xs, xst

    def finish():
        raise _Stop()

    with ExitStack() as ph:
        hnT = ph.enter_context(nc.sbuf_tensor("hnT", [128, 8, S], BF16))
        hn_tok = [Tok() for _ in range(NT)]
        if True:
            p1 = ph
            wB = p1.enter_context(nc.sbuf_tensor("wB1", [128, D], F32))
            wBt = Tok()
            K.dma("sp", wB[:], nw_d["nw_mix"].partition_broadcast(128), K.slot("wB"), [], [wBt])
            xr = Rot(nc, p1, "xt", [128, D], F32, 4)
            tmp = {"junk": Rot(nc, p1, "junk", [128, D], BF16, 2), "st": Rot(nc, p1, "st", [128, 4], F32, 4),
                   "xs": Rot(nc, p1, "xs", [128, D], BF16, 2)}
            ndone = [0]
            nload = [0]
            xq = []

            def need(t_hi):
                while ndone[0] < t_hi:
                    while nload[0] < min(NT, ndone[0] + 3):
                        t = nload[0]
                        xt, xtok = xr.get()
                        K.dma("sp", xt[:], x_d[t * 128:(t + 1) * 128, :], K.slot("xt%d" % (t % 4)), [], [xtok])
                        xq.append((xt, xtok))
                        nload[0] += 1
                    t = ndone[0]
                    xt, xtok = xq.pop(0)
                    norm_tile(xt[:], xtok, wB[:], wBt, hnT[:, :, t * 128:(t + 1) * 128], hn_tok[t], tmp)
                    ndone[0] += 1
        if "hnT" in dbg:
            need(NT)
            hd = nc.dram_tensor("hnT_dbg", [128, 8, S], BF16, kind="ExternalOutput").ap()
            K.dma("sp", hd, hnT[:], K.slot("dbg"), hn_tok, [])
        wblk = Rot(nc, ph, "wblk", [128, 8, 512], BF16, 2)
        stgF = Rot(nc, ph, "stgF", [128, S], F32, 2)
        stgB = Rot(nc, ph, "stgB", [128, S], BF16, 2)
        stgT = Rot(nc, ph, "stgT", [128, 512], BF16, 4)
        ev = [0]

        def load_w(c0, n):
            wb_, wt = wblk.get()
            i = wblk.i
            K.dma("pool", wb_[:, :, 0:n], w_in_d[:, c0:c0 + n].rearrange("(kc p) c -> p kc c", p=128),
                  K.slot("wblk%d" % i), [], [wt])
            return wb_, wt

        def fm_block(c0, n, dst, dtok, drow0, func, obf):
            wb_, wt = load_w(c0, n)
            for cc in range((n + 127) // 128):
                m = min(128, n - cc * 128)
                stg, stok = (stgB if obf else stgF).get()
                si = (stgB if obf else stgF).i
                for tt in range(8):
                    need(tt * 4 + 4)
                    ps, pst = PS.get()
                    for kc in range(8):
                        K.mm(ps[0:m, :], wb_[:, kc, cc * 128:cc * 128 + m], hnT[:, kc, tt * 512:(tt + 1) * 512],
                             kc == 0, kc == 7, [wt] + hn_tok[tt * 4:tt * 4 + 4], [pst])
                    if func is not None:
                        K.act(stg[0:m, tt * 512:(tt + 1) * 512], ps[0:m, :], func, [pst], [stok])
                    else:
                        ev[0] ^= 1
                        K.cp("act" if ev[0] else "dve", stg[0:m, tt * 512:(tt + 1) * 512], ps[0:m, :], [pst], [stok])
                K.dma("sp", dst[drow0 + cc * 128:drow0 + cc * 128 + m, :], stg[0:m, :],
                      K.slot(("sB%d" if obf else "sF%d") % si), [stok], [dtok])

        def tm_block(c0, dst, dtok, dcol0, func):
            wb_, wt = load_w(c0, 512)
            for t in range(NT):
                need(t + 1)
                ps, pst = PS.get()
                for kc in range(8):
                    K.mm(ps[:], hnT[:, kc, t * 128:(t + 1) * 128], wb_[:, kc, :], kc == 0, kc == 7,
                         [wt, hn_tok[t]], [pst])
                stg, stok = stgT.get()
                si = stgT.i
                K.act(stg[:], ps[:], func, [pst], [stok])
                K.dma("sp", dst[t * 128:(t + 1) * 128, dcol0:dcol0 + 512], stg[:], K.slot("sT%d" % si), [stok], [dtok])

        for b in range(6):
            fm_block(b * 512, 512, qkvT_s, DT["qkvT"], b * 512, None, True)
        fm_block(3072, 16, abT_s, DT["abT"], 0, None, False)
        for b in range(2):
            tm_block(3088 + b * 512, ogA_s, DT["ogA"], b * 512, AF.Silu)
        for b in range(2):
            fm_block(4112 + b * 512, 512, fT_s, DT["fT"], b * 512, None, True)
        for b in range(2):
            fm_block(5136 + b * 512, 512, qbT_s, DT["qbT"], b * 512, None, True)
        for b in range(2):
            tm_block(6160 + b * 512, ib_s, DT["ib"], b * 512, AF.Copy)
        for b in range(2):
            tm_block(7184 + b * 512, ogB_s, DT["ogB"], b * 512, AF.Silu)
        for b in range(2):
            fm_block(8208 + b * 512, 512, sgAT_s, DT["sgAT"], b * 512, AF.Sigmoid, True)
        for b in range(2):
            fm_block(9232 + b * 512, 512, sgBT_s, DT["sgBT"], b * 512, AF.Sigmoid, True)
        K.barrier()
    if stop == "P2":
        return finish()

    qT_s = scratch("qT", [D, S], BF16)
    qdT_s = scratch("qdT", [D, S], BF16)
    kT_s = scratch("kT", [D, S], BF16)
    kdec_s = scratch("kdec", [S, D], BF16)
    kbdec_s = scratch("kbdec", [S, D], BF16)
    vb_s = scratch("vb", [S, D], BF16)
    yaT_s = scratch("yaT", [D, S], BF16)
    ybT_s = scratch("ybT", [D, S], BF16)
    cd_s = scratch("cd", [8, 64], F32)
    cum_s = scratch("cumrows", [8, S], F32)
    DT["cumrows"] = Tok(nowaw=True)
    oa_s = scratch("o_a", [S, D], F32) if "o_a" in dbg else None
    for n in ("qT", "qdT", "kT", "kdec", "kbdec", "vb", "yaT", "ybT", "cd", "o_a"):
        DT[n] = Tok(nowaw=True)
    DKS = 128 ** -0.5

    with ExitStack() as ph:
        cum = ph.enter_context(nc.sbuf_tensor("g_cum", [8, S], F32))
        cumt = Tok()
        tsc = {n: ph.enter_context(nc.sbuf_tensor("tsc_" + n, [128, NT, 8], F32)) for n in ("elmc", "bec", "be", "nb", "cum")}
        tsct = Tok()
        cdB = ph.enter_context(nc.sbuf_tensor("cdB", [128, 512], F32))
        cdBt = Tok()
        gwB = ph.enter_context(nc.sbuf_tensor("gwB", [128, 128], F32))
        gwt = Tok()
        K.dma("sp", gwB[:], gnw_d.partition_broadcast(128), K.slot("gwB"), [], [gwt])
        cwt = ph.enter_context(nc.sbuf_tensor("g_cw", [128, 24, 4], F32))
        cwtt = Tok()
        K.dma("sp", cwt[:], cw_d, K.slot("cw"), [], [cwtt])
        with ExitStack() as p3:
            ecum = p3.enter_context(nc.sbuf_tensor("g_ecum", [8, S], F32))
            ecumt = Tok()
            with ExitStack() as p3a:
                A = p3a.enter_context(nc.sbuf_tensor("g_A", [8, S], F32))
                Bt = p3a.enter_context(nc.sbuf_tensor("g_B", [8, S], F32))
                Tm = p3a.enter_context(nc.sbuf_tensor("g_T", [8, S], F32))
                rm = p3a.enter_context(nc.sbuf_tensor("g_rm", [8, S], F32))
                sc8 = p3a.enter_context(nc.sbuf_tensor("g_sc8", [8, 4], F32))
                cdr = p3a.enter_context(nc.sbuf_tensor("g_cdr", [8, 64], F32))
                At, Btt, Tmt, rmt, sct, cdrt = Tok(), Tok(), Tok(), Tok(), Tok(), Tok()
                sl = K.slot("g3a")
                K.dma("sp", A[:], abT_s[0:8, :], K.slot("g3a_A"), [DT["abT"]], [At])
                K.dma("sp", Bt[:], abT_s[8:16, :], K.slot("g3a_B"), [DT["abT"]], [Btt])
                K.dma("sp", rm[:], rmask_d[0:8, :], K.slot("g3a_rm"), [], [rmt])
                K.dma("sp", sc8[:, 0:1], alog_d, K.slot("g3a_sc"), [], [sct])
                K.dma("sp", sc8[:, 1:2], dtb_d, K.slot("g3a_sc"), [], [sct])
                K.act(sc8[:, 2:3], sc8[:, 0:1], AF.Exp, [sct], [sct])
                K.ts("dve", sc8[:, 3:4], sc8[:, 2:3], -1.0, None, ALU.mult, None, [sct], [sct])
                K.act(A[:], A[:], AF.Exp, [At, sct], [At], bias=sc8[:, 1:2])
                K.act(A[:], A[:], AF.Ln, [At, ctok], [At], bias=C.onec[0:8, 0:1])
                K.ts("dve", A[:], A[:], sc8[:, 3:4], None, ALU.mult, None, [At, sct], [At])
                K.op("dve", lambda e: e.tensor_tensor_scan(out=cum[:], data0=rm[:], data1=A[:], initial=0.0,
                                                           op0=ALU.mult, op1=ALU.add), [At, rmt], [cumt])
                K.act(Bt[:], Bt[:], AF.Sigmoid, [Btt], [Btt])
                K.act(ecum[:], cum[:], AF.Exp, [cumt], [ecumt])
                cum3 = cum[:].rearrange("p (c t) -> p c t", t=64)
                K.tt("dve", A[:].rearrange("p (c t) -> p c t", t=64), cum3[:, :, 63:64].to_broadcast([8, 64, 64]), cum3,
                     ALU.subtract, [cumt, At], [At])
                K.act(A[:], A[:], AF.Exp, [At], [At])
                K.cp("dve", cdr[:], ecum[:].rearrange("p (c t) -> p c t", t=64)[:, :, 63], [ecumt], [cdrt])
                K.dma("sp", cd_s, cdr[:], K.slot("g3a_cd"), [cdrt], [DT["cd"]])
                K.dma("sp", cdB[:], cd_s.rearrange("h c -> (h c)").partition_broadcast(128), K.slot("g3a_cdB"), [DT["cd"]], [cdBt])

                def to_tok(src, srct, name):
                    ps, pst = PS.get()
                    for b in range(NT):
                        K.tr(ps[:, b * 8:(b + 1) * 8], src[0:8, b * 128:(b + 1) * 128], C.identf[0:8, 0:8], [srct, ctok], [pst])
                    K.cp("act", tsc[name][:].rearrange("p b h -> p (b h)"), ps[:, 0:256], [pst], [tsct])

                to_tok(A, At, "elmc")
                to_tok(cum, cumt, "cum")
                K.dma("sp", cum_s, cum[:], K.slot("g3a_cum"), [cumt], [DT["cumrows"]])
                to_tok(Bt, Btt, "be")
                K.tt("dve", Tm[:], Bt[:], ecum[:], ALU.mult, [Btt, ecumt], [Tmt])
                to_tok(Tm, Tmt, "bec")
                K.ts("dve", Tm[:], Bt[:], -1.0, None, ALU.mult, None, [Btt, Tmt], [Tmt])
                to_tok(Tm, Tmt, "nb")
                K.barrier()
            pre_r = Rot(nc, p3, "g_pre", [128, S + 3], BF16, 3)
            for i_ in range(3):
                K.op("pool", lambda e: e.memset(pre_r.bufs[i_][:, 0:3], 0.0), [], [pre_r.toks[i_]])
            dg_r = Rot(nc, p3, "g_dg", [128, 4, 128], BF16, 3)
            fm_r = Rot(nc, p3, "g_fm", [128, S], BF16, 5)
            tm_r = Rot(nc, p3, "g_tm", [128, NT, 128], BF16, 4)
            sq_r = Rot(nc, p3, "g_sq", [128, 512], BF16, 4)
            sda_r = Rot(nc, p3, "g_sda", [128, S], F32, 2)
            cnt = [0]

            def store_fm(dst, name, h, buf, bt):
                i = [j for j, b_ in enumerate(fm_r.bufs) if b_ is buf][0]
                K.dma("sp", dst[h * 128:(h + 1) * 128, :], buf[:], K.slot("gfm%d" % i), [bt], [DT[name]])

            def store_tm(dst, name, h, buf, bt):
                i = [j for j, b_ in enumerate(tm_r.bufs) if b_ is buf][0]
                for g8 in range(4):
                    K.dma("sp", dst[g8 * 1024:(g8 + 1) * 1024, h * 128:(h + 1) * 128].rearrange("(b p) d -> p b d", p=128),
                          buf[:, g8 * 8:(g8 + 1) * 8, :], K.slot("gtm%d" % i), [bt], [DT[name]])

            def transposes(srcT, srct, outs, h):
                for g8 in range(4):
                    ps, pst = PS.get()
                    psb = ps[:].bitcast(BF16)
                    for j in range(8):
                        b = g8 * 8 + j
                        K.tr(psb[:, j * 128:(j + 1) * 128], srcT[:, b * 128:(b + 1) * 128], C.identb[:], [srct, ctok], [pst])
                    for (nm, buf, bt) in outs:
                        K.tt("dve", buf[:, g8 * 8:(g8 + 1) * 8, :], psb.rearrange("p (b d) -> p b d", b=8),
                             tsc[nm][:, g8 * 8:(g8 + 1) * 8, h:h + 1].to_broadcast([128, 8, 128]), ALU.mult,
                             [pst, tsct], [bt])

            def tile_gen(h, si, tt, pre, pret, dg, dgt, outs, sda, sdat):
                cs_ = slice(tt * 512, (tt + 1) * 512)
                ps, pst = PS.get()
                for j in range(4):
                    K.mm(ps[:], dg[:, j, :], pre[:, tt * 512 + j:tt * 512 + j + 512], j == 0, j == 3, [dgt, pret], [pst])
                xT, xTt = outs[0], outs[1]
                K.act(xT[:, cs_], ps[:], AF.Silu, [pst], [xTt])
                if si == 2:
                    return
                yield
                sq, sqt = sq_r.get()
                K.act(sq[:], xT[:, cs_], AF.Square, [xTt], [sqt])
                ps1, ps1t = PS.get(hold=True)
                K.mm(ps1[:], C.onesb[:], sq[:], True, True, [sqt, ctok], [ps1t])
                yield
                K.ts("dve", sda[:, cs_], ps1[:], EPS, None, ALU.add, None, [ps1t], [sdat])
                PS.rel(ps1)
                yield

            def tail_gen(h, si, outs, sda, sdat):
                for _ in range(4):
                    yield
                if si < 2:
                    K.act(sda[:], sda[:], AF.Ln, [sdat], [sdat])
                    K.act(sda[:], sda[:], AF.Exp, [sdat], [sdat], scale=-0.5)
                    yield
                if si == 0:
                    qT, qTt, qdT, qdTt = outs
                    K.stt("dve", qT[:], qT[:], DKS, sda[:], ALU.mult, ALU.mult, [qTt, sdat], [qTt])
                    for tt in range(8):
                        cs_ = slice(tt * 512, (tt + 1) * 512)
                        ps2, ps2t = PS.get()
                        K.mm(ps2[:], C.selh[0:8, h, :], ecum[0:8, cs_], True, True, [ecumt, ctok], [ps2t])
                        K.tt("dve", qdT[:, cs_], qT[:, cs_], ps2[:], ALU.mult, [qTt, ps2t], [qdTt])
                    store_fm(qT_s, "qT", h, qT, qTt)
                    store_fm(qdT_s, "qdT", h, qdT, qdTt)
                elif si == 1:
                    kT, kTt = outs
                    K.tt("dve", kT[:], kT[:], sda[:], ALU.mult, [kTt, sdat], [kTt])
                    store_fm(kT_s, "kT", h, kT, kTt)
                    yield
                    kd, kdt = tm_r.get()
                    kb, kbt = tm_r.get()
                    transposes(kT, kTt, [("elmc", kd, kdt), ("bec", kb, kbt)], h)
                    store_tm(kdec_s, "kdec", h, kd, kdt)
                    store_tm(kbdec_s, "kbdec", h, kb, kbt)
                else:
                    vT, vTt = outs
                    vbst, vbt = tm_r.get()
                    transposes(vT, vTt, [("be", vbst, vbt)], h)
                    store_tm(vb_s, "vb", h, vbst, vbt)
                yield

            def all_gens():
                for h in range(8):
                    for si in range(3):
                        c = si * 8 + h
                        pre, pret = pre_r.get()
                        K.dma("sp", pre[:, 3:S + 3], qkvT_s[si * 1024 + h * 128: si * 1024 + (h + 1) * 128, :],
                              K.slot("gpre%d" % pre_r.i), [DT["qkvT"]], [pret])
                        dg, dgt = dg_r.get()
                        for j in range(4):
                            K.ts("pool", dg[:, j, :], C.identb[:], cwt[:, c, j:j + 1], None, ALU.mult, None, [ctok, cwtt], [dgt])
                        if si == 0:
                            outs = fm_r.get() + fm_r.get()
                        else:
                            outs = fm_r.get()
                        sda, sdat = sda_r.get() if si < 2 else (None, None)
                        for tt in range(8):
                            yield tile_gen(h, si, tt, pre, pret, dg, dgt, outs, sda, sdat)
                        yield tail_gen(h, si, outs, sda, sdat)

            run_skewed(all_gens(), int(os.environ.get("SKW", "5")))
            K.barrier()
        if stop == "P3":
            raise _Stop()

        def finish_y(px, o_blk, obt, wB_, wt_, og3, ogt_, dst_s, dname, b, R):
            sqj, sqt_ = R["sqj"].get()
            K.tt("pool", sqj[:], o_blk[:], o_blk[:], ALU.mult, [obt], [sqt_])
            ss, sst = R["ss"].get()
            K.op("dve", lambda e: e.tensor_reduce(out=ss[:], in_=sqj[:], axis=AX.X, op=ALU.add), [sqt_], [sst])
            K.act(ss[:], ss[:], AF.Sqrt, [sst, ctok], [sst], scale=1.0 / 128, bias=C.epsc[:, 0:1])
            K.op("dve", lambda e: e.reciprocal(out=ss[:], in_=ss[:]), [sst], [sst])
            K.tt("dve", sqj[:], o_blk[:], ss[:].unsqueeze(2).to_broadcast([128, 8, 128]), ALU.mult, [obt, sst, sqt_], [sqt_])
            K.tt("pool", sqj[:], sqj[:], wB_[:].unsqueeze(1).to_broadcast([128, 8, 128]), ALU.mult, [sqt_, wt_], [sqt_])
            yb, ybt = R["yb"].get()
            K.tt("pool", yb[:], sqj[:], og3, ALU.mult, [sqt_, ogt_], [ybt])
            ps, pst = PS.get()
            psb = ps[:].bitcast(BF16)
            for h in range(8):
                K.tr(psb[:, h * 128:(h + 1) * 128], yb[:, h, :], C.identb[:], [ybt, ctok], [pst])
            yT, yTt = R["yT"].get()
            K.cp("act", yT[:], psb.rearrange("p (h t) -> p h t", h=8), [pst], [yTt])
            K.dma("sp", dst_s[:, b * 128:(b + 1) * 128].rearrange("(h p) t -> p h t", p=128), yT[:],
                  K.slot("%s_yT%d" % (px, R["yT"].i)), [yTt], [DT[dname]])

        with ExitStack() as p4:
            names = ("qT", "qdT", "kT", "kdec", "kbdec", "vb", "ogA")
            ld_r = {n: Rot(nc, p4, "l_" + n, [128, 1024], BF16, 3) for n in names}
            cr_r = Rot(nc, p4, "l_cr", [128, 8, 128], F32, 3)
            dm_r = Rot(nc, p4, "g_dm", [128, 2, 512], F32, 2)
            tmp_r = Rot(nc, p4, "g_tmp", [128, 512], F32, 2)
            n0_r = Rot(nc, p4, "g_n0", [128, 512], F32R, 2)
            n0T_r = Rot(nc, p4, "g_n0T", [128, 512], F32R, 2)
            m_r = Rot(nc, p4, "g_m", [128, 2, 512], F32R, 4)
            t_r = Rot(nc, p4, "g_t", [128, 512], F32R, 4)
            tb_r = Rot(nc, p4, "g_tb", [128, 512], BF16, 4)
            qk_r = Rot(nc, p4, "g_qk", [128, 512], BF16, 4)
            nw_r = Rot(nc, p4, "g_nw", [128, 512], BF16, 4)
            vn_r = Rot(nc, p4, "g_vn", [128, 512], BF16, 2)
            ob_r = Rot(nc, p4, "g_ob", [128, 8, 128], F32, 2)
            YR = {"sqj": Rot(nc, p4, "g_sqj", [128, 8, 128], F32, 1), "ss": Rot(nc, p4, "g_ss", [128, 8], F32, 2),
                  "yb": Rot(nc, p4, "g_yb", [128, 8, 128], BF16, 2), "yT": Rot(nc, p4, "g_yT", [128, 8, 128], BF16, 2)}
            S32 = p4.enter_context(nc.sbuf_tensor("g_S32", [128, 8, 128], F32))
            Sb = p4.enter_context(nc.sbuf_tensor("g_Sb", [128, 8, 128], BF16))
            s32t = [Tok(), Tok()]
            sbt = [Tok(), Tok()]
            for g in range(2):
                K.op("dve", lambda e: e.memset(S32[:, 4 * g:4 * g + 4, :], 0.0), [], [s32t[g]])
                K.op("dve", lambda e: e.memset(Sb[:, 4 * g:4 * g + 4, :], 0.0), [], [sbt[g]])
            cdB3 = cdB[:].rearrange("p (h c) -> p h c", c=64)
            H3 = lambda ap: ap.rearrange("p (h j) -> p h j", h=4)

            def load_block(b):
                bufs = {}
                ldt = Tok()
                bc = slice(b * 128, (b + 1) * 128)
                for n in names:
                    buf, _ = ld_r[n].get()
                    bufs[n] = buf
                i = ld_r["qT"].i
                sl_ = K.slot("gld%d" % i)
                prev = ld_r["qT"].toks[i]
                for n, src in (("qT", qT_s), ("qdT", qdT_s), ("kT", kT_s)):
                    K.dma("sp", bufs[n][:].rearrange("p (h t) -> p h t", h=8), src[:, bc].rearrange("(h p) t -> p h t", p=128),
                          sl_, [DT[n]], [prev])
                for n, src in (("kdec", kdec_s), ("kbdec", kbdec_s), ("vb", vb_s), ("ogA", ogA_s)):
                    K.dma("sp", bufs[n][:], src[bc, :], sl_, [DT[n]], [prev])
                crb, _ = cr_r.get()
                bufs["cr"] = crb
                for h_ in range(8):
                    K.dma("sp", crb[:, h_, :], cum_s[h_:h_ + 1, bc].partition_broadcast(128), sl_, [DT["cumrows"]], [prev])
                return bufs, prev

            def run_lockstep(gens):
                gens = list(gens)
                while gens:
                    for gg in list(gens):
                        try:
                            next(gg)
                        except StopIteration:
                            gens.remove(gg)

            prods = {}

            def front(b, g, L, ldt):
                bc = slice(b * 128, (b + 1) * 128)
                qTb = L["qT"][:].rearrange("p (h t) -> p h t", h=8)
                kTb = L["kT"][:].rearrange("p (h t) -> p h t", h=8)
                kbb = L["kbdec"]
                hs = list(range(4 * g, 4 * g + 4))
                psG, tG = PS.get()
                psQK, tQK = PS.get()
                for hl, h in enumerate(hs):
                    c = slice(hl * 128, (hl + 1) * 128)
                    K.mm(psG[:, c], kTb[:, h, :], kTb[:, h, :], True, True, [ldt], [tG])
                    K.mm(psQK[:, c], kTb[:, h, :], qTb[:, h, :], True, True, [ldt], [tQK])
                dm, dmt = dm_r.get()
                crb = L["cr"]
                for hl, h in enumerate(hs):
                    c = slice(hl * 128, (hl + 1) * 128)
                    K.stt("dve", dm[:, 0, c], crb[:, h, :], tsc["cum"][:, b, h:h + 1], C.mnegS[:], ALU.subtract, ALU.add,
                          [ldt, tsct, ctok], [dmt])
                    K.stt("dve", dm[:, 1, c], crb[:, h, :], tsc["cum"][:, b, h:h + 1], C.mnegIT[:], ALU.subtract, ALU.add,
                          [ldt, tsct, ctok], [dmt])
                K.act(dm[:, 0, :], dm[:, 0, :], AF.Exp, [dmt], [dmt], scale=-1.0)
                K.act(dm[:, 1, :], dm[:, 1, :], AF.Exp, [dmt], [dmt])
                tmpN, tnt = tmp_r.get()
                K.tt("dve", H3(tmpN[:]), H3(psG[:]), tsc["nb"][:, b, 4 * g:4 * g + 4].unsqueeze(2).to_broadcast([128, 4, 128]),
                     ALU.mult, [tG, tsct], [tnt])
                N0, n0t = n0_r.get()
                K.tt("dve", N0[:], tmpN[:], dm[:, 0, :], ALU.mult, [tnt, dmt], [n0t])
                qkT, qkt = qk_r.get()
                K.tt("dve", qkT[:], psQK[:], dm[:, 1, :], ALU.mult, [tQK, dmt], [qkt])
                yield
                psT, tT = PS.get()
                for hl in range(4):
                    c = slice(hl * 128, (hl + 1) * 128)
                    K.tr(psT[:, c], N0[:, c].bitcast(F32), C.identf[:], [n0t, ctok], [tT])
                N0T, n0Tt = n0T_r.get()
                K.cp("act", N0T[:], psT[:], [tT], [n0Tt])
                Tt, ttt = t_r.get()
                K.tt("dve", H3(Tt[:]), H3(N0T[:].bitcast(F32)), C.identf[:].unsqueeze(1).to_broadcast([128, 4, 128]), ALU.add,
                     [n0Tt, ctok], [ttt])
                yield
                M, Mtk, MT, MTtk = N0[:], n0t, N0T[:], n0Tt
                for lvl in range(1, 6):
                    last = (lvl == 5)
                    psM, tM = PS.get()
                    for hl in range(4):
                        c = slice(hl * 128, (hl + 1) * 128)
                        K.mm(psM[:, c], MT[:, c], M[:, c], True, True, [Mtk, MTtk], [tM])
                    if not last:
                        psMT, tMT = PS.get()
                        for hl in range(4):
                            c = slice(hl * 128, (hl + 1) * 128)
                            K.mm(psMT[:, c], M[:, c], MT[:, c], True, True, [Mtk, MTtk], [tMT])
                    Mn, Mnt = m_r.get()
                    K.cp("act", Mn[:, 0, :], psM[:], [tM], [Mnt])
                    if not last:
                        K.cp("act", Mn[:, 1, :], psMT[:], [tMT], [Mnt])
                    yield
                    psP, tP = PS.get()
                    for hl in range(4):
                        c = slice(hl * 128, (hl + 1) * 128)
                        K.mm(psP[:, c], Mn[:, 0, c], Tt[:, c], True, True, [Mnt, ttt], [tP])
                    if not last:
                        Tn, tnn = t_r.get()
                        K.tt("dve", Tn[:], Tt[:].bitcast(F32), psP[:], ALU.add, [ttt, tP], [tnn])
                        Tt, ttt = Tn, tnn
                        M, Mtk, MT, MTtk = Mn[:, 0, :], Mnt, Mn[:, 1, :], Mnt
                    else:
                        Ttb, ttbt = tb_r.get()
                        K.tt("dve", Ttb[:], Tt[:].bitcast(F32), psP[:], ALU.add, [ttt, tP], [ttbt])
                    yield
                psW, tW = PS.get()
                for hl, h in enumerate(hs):
                    c = slice(hl * 128, (hl + 1) * 128)
                    K.mm(psW[:, c], kbb[:, h * 128:(h + 1) * 128], Ttb[:, c], True, True, [ldt, ttbt], [tW])
                nwT, nwt = nw_r.get()
                K.act(nwT[:], psW[:], AF.Copy, [tW], [nwt], scale=-1.0)
                prods[(b, g)] = (Ttb, ttbt, qkT, qkt, nwT, nwt)
                yield

            def back(b, g, L, ldt, o_blk, obt):
                Ttb, ttbt, qkT, qkt, nwT, nwt = prods.pop((b, g))
                qdTb = L["qdT"][:].rearrange("p (h t) -> p h t", h=8)
                kdb, vbb = L["kdec"], L["vb"]
                hs = list(range(4 * g, 4 * g + 4))
                for ci in range(2):
                    r = slice(ci * 64, ci * 64 + 64)
                    psV, tV = PS.get()
                    for hl, h in enumerate(hs):
                        c = slice(hl * 128, (hl + 1) * 128)
                        ic = slice(hl * 128 + ci * 64, hl * 128 + ci * 64 + 64)
                        K.mm(psV[r, c], Ttb[:, ic], vbb[:, h * 128:(h + 1) * 128], True, False, [ttbt, ldt], [tV])
                        K.mm(psV[r, c], nwT[:, ic], Sb[:, h, :], False, True, [nwt, sbt[g]], [tV])
                    vn, vnt = vn_r.get()
                    K.cp("act", vn[r, :], psV[r, :], [tV], [vnt])
                    yield
                    psO, tO = PS.get()
                    for hl, h in enumerate(hs):
                        c = slice(hl * 128, (hl + 1) * 128)
                        ic = slice(hl * 128 + ci * 64, hl * 128 + ci * 64 + 64)
                        K.mm(psO[r, c], qdTb[:, h, ci * 64:ci * 64 + 64], Sb[:, h, :], True, False, [ldt, sbt[g]], [tO])
                        K.mm(psO[r, c], qkT[r, ic], vn[r, c], False, True, [qkt, vnt], [tO])
                    K.cp("act", o_blk[r, 4 * g:4 * g + 4, :], H3(psO[r, :]), [tO], [obt])
                    psS, tS = PS.get()
                    for hl, h in enumerate(hs):
                        c = slice(hl * 128, (hl + 1) * 128)
                        K.mm(psS[:, c], kdb[r, h * 128:(h + 1) * 128], vn[r, c], True, True, [ldt, vnt], [tS])
                    ch = 2 * b + ci
                    Sg = S32[:, 4 * g:4 * g + 4, :]
                    K.tt("dve", Sg, Sg, cdB3[:, 4 * g:4 * g + 4, ch:ch + 1].to_broadcast([128, 4, 128]), ALU.mult,
                         [s32t[g], cdBt], [s32t[g]])
                    K.tt("dve", Sg, Sg, H3(psS[:]), ALU.add, [s32t[g], tS], [s32t[g]])
                    K.cp("act", Sb[:, 4 * g:4 * g + 4, :], Sg, [s32t[g]], [sbt[g]])
                    yield

            blocks = {0: load_block(0)}
            if NT > 1:
                blocks[1] = load_block(1)
            run_lockstep([front(0, 0, *blocks[0]), front(0, 1, *blocks[0])])
            for b in range(NT):
                L, ldt = blocks[b]
                if b + 2 < NT:
                    blocks[b + 2] = load_block(b + 2)
                bc = slice(b * 128, (b + 1) * 128)
                o_blk, obt = ob_r.get()
                gens = [back(b, 0, L, ldt, o_blk, obt)]
                if b + 1 < NT:
                    gens.append(front(b + 1, 0, *blocks[b + 1]))
                gens.append(back(b, 1, L, ldt, o_blk, obt))
                if b + 1 < NT:
                    gens.append(front(b + 1, 1, *blocks[b + 1]))
                run_lockstep(gens)
                if oa_s is not None:
                    K.dma("sp", oa_s[bc, :], o_blk[:].rearrange("p h d -> p (h d)"), K.slot("oa%d" % ob_r.i), [obt], [DT["o_a"]])
                finish_y("a", o_blk, obt, gwB, gwt, L["ogA"][:].rearrange("p (h d) -> p h d", h=8), ldt, yaT_s, "yaT", b, YR)
                del blocks[b]
            K.barrier()
        if stop == "P4":
            raise _Stop()

    lgall = es.enter_context(nc.sbuf_tensor("t_lgall", [128, NT, 36], F32))
    lgt = Tok()
    qinT_s = scratch("qinT", [D, S], BF16)
    kinT_s = scratch("kinT", [D, S], BF16)
    kdb_s = scratch("kdecb", [S, D], BF16)
    ob_s = scratch("o_b", [S, D], F32) if "o_b" in dbg else None
    for n in ("qinT", "kinT", "kdecb", "o_b"):
        DT[n] = Tok(nowaw=True)
    with ExitStack() as ph:
        elast = es.enter_context(nc.sbuf_tensor("h_elast", [128, 8, 64], F32))
        elt = Tok()
        hwB = es.enter_context(nc.sbuf_tensor("hwB", [128, 128], F32))
        hwt = Tok()
        K.dma("sp", hwB[:], hnw_d.partition_broadcast(128), K.slot("hwB"), [], [hwt])
        lbt_ = es.enter_context(nc.sbuf_tensor("h_lbt", [128, 2, 8], F32))
        lb = es.enter_context(nc.sbuf_tensor("h_lb", [128, 8], F32))
        oml = es.enter_context(nc.sbuf_tensor("h_oml", [128, 8], F32))
        lbtok = Tok()
        K.dma("sp", lbt_[:], lb_d, K.slot("lbt"), [], [lbtok])
        K.tt("dve", lb[:], lbt_[:, 0, :], lbt_[:, 1, :], ALU.subtract, [lbtok], [lbtok])
        K.act(lb[:], lb[:], AF.Sigmoid, [lbtok], [lbtok])
        K.ts("dve", oml[:], lb[:], -1.0, 1.0, ALU.mult, ALU.add, [lbtok], [lbtok])
        with ExitStack() as p5:
            PW = 2048
            hrm = p5.enter_context(nc.sbuf_tensor("h_rm", [128, PW], F32))
            hrmt = Tok()
            K.dma("sp", hrm[:], rmask_d[:, 0:PW], K.slot("hrm"), [], [hrmt])
            hf_r = Rot(nc, p5, "h_f", [128, PW], F32, 3)
            hfl_r = Rot(nc, p5, "h_fl", [128, PW], BF16, 3)
            hq_r = Rot(nc, p5, "h_q", [128, PW], BF16, 3)
            h2_r = Rot(nc, p5, "h_b2", [128, PW], F32, 3)
            h3_r = Rot(nc, p5, "h_b3", [128, PW], F32, 3)
            h4_r = Rot(nc, p5, "h_b4", [128, PW], F32, 3)
            hqo_r = Rot(nc, p5, "h_qo", [128, PW], BF16, 3)
            hko_r = Rot(nc, p5, "h_ko", [128, PW], BF16, 3)
            hkd_r = Rot(nc, p5, "h_kd", [128, PW], BF16, 3)
            hkt_r = Rot(nc, p5, "h_kt", [128, PW // 128, 128], BF16, 3)
            hcnt = [0]

            def hslot(px):
                hcnt[0] = (hcnt[0] + 1) % 6
                return K.slot("%s%d" % (px, hcnt[0]))

            def hpiece(h, pc):
                cs_ = slice(pc * PW, (pc + 1) * PW)
                nch = PW // 64
                fl, flt = hfl_r.get()
                K.dma("sp", fl[:], fT_s[h * 128:(h + 1) * 128, cs_], K.slot("hlf%d" % hfl_r.i), [DT["fT"]], [flt])
                qb, qbt = hq_r.get()
                K.dma("sp", qb[:], qbT_s[h * 128:(h + 1) * 128, cs_], K.slot("hlq%d" % hq_r.i), [DT["qbT"]], [qbt])
                yield
                fb, fbt = hf_r.get()
                K.act(fb[:], fl[:], AF.Sigmoid, [flt], [fbt])
                yield
                K.ts("dve", fb[:], fb[:], oml[:, h:h + 1], lb[:, h:h + 1], ALU.mult, ALU.add, [fbt, lbtok], [fbt])
                yield
                b2, b2t = h2_r.get()
                K.act(b2[:], fb[:], AF.Ln, [fbt], [b2t])
                yield
                b3, b3t = h3_r.get()
                K.op("dve", lambda e: e.tensor_tensor_scan(out=b3[:], data0=hrm[:], data1=b2[:], initial=0.0,
                                                           op0=ALU.mult, op1=ALU.add), [b2t, hrmt], [b3t])
                yield
                b4, b4t = h4_r.get()
                K.act(b4[:], b3[:], AF.Exp, [b3t], [b4t])
                K.act(b2[:], b3[:], AF.Exp, [b3t, b2t], [b2t], scale=-1.0)
                yield
                el_ = elast[:, h, pc * nch:(pc + 1) * nch]
                K.cp("dve", el_, b4[:].rearrange("p (c t) -> p c t", t=64)[:, :, 63], [b4t], [elt])
                qin, qint = hqo_r.get()
                K.stt("dve", qin[:], qb[:], DKS, b4[:], ALU.mult, ALU.mult, [qbt, b4t], [qint])
                K.ts("pool", fb[:], fb[:], -1.0, 1.0, ALU.mult, ALU.add, [fbt], [fbt])
                K.tt("pool", b2[:], fb[:], b2[:], ALU.mult, [fbt, b2t], [b2t])
                yield
                kin, kint = hko_r.get()
                K.cp("act", kin[:], b2[:], [b2t], [kint])
                kdT, kdTt = hkd_r.get()
                K.tt("dve", kdT[:].rearrange("p (c t) -> p c t", t=64), b2[:].rearrange("p (c t) -> p c t", t=64),
                     el_.unsqueeze(2).to_broadcast([128, nch, 64]), ALU.mult, [b2t, elt], [kdTt])
                K.dma("sp", qinT_s[h * 128:(h + 1) * 128, cs_], qin[:], K.slot("hsq%d" % hqo_r.i), [qint], [DT["qinT"]])
                K.dma("sp", kinT_s[h * 128:(h + 1) * 128, cs_], kin[:], K.slot("hsk%d" % hko_r.i), [kint], [DT["kinT"]])
                yield
                nb_ = PW // 128
                kd, kdt = hkt_r.get()
                for g8 in range(nb_ // 8):
                    ps, pst = PS.get()
                    psb = ps[:].bitcast(BF16)
                    for j in range(8):
                        bb = g8 * 8 + j
                        K.tr(psb[:, j * 128:(j + 1) * 128], kdT[:, bb * 128:(bb + 1) * 128], C.identb[:], [kdTt, ctok], [pst])
                    K.cp("act", kd[:, g8 * 8:(g8 + 1) * 8, :], psb.rearrange("p (b d) -> p b d", b=8), [pst], [kdt])
                for g8 in range(nb_ // 8):
                    r0 = pc * PW + g8 * 1024
                    K.dma("sp", kdb_s[r0:r0 + 1024, h * 128:(h + 1) * 128].rearrange("(b p) d -> p b d", p=128),
                          kd[:, g8 * 8:(g8 + 1) * 8, :], K.slot("hsd%d" % hkt_r.i), [kdt], [DT["kdecb"]])
                yield

            run_skewed((hpiece(h, pc) for h in range(8) for pc in range(S // PW)), 3)
            K.barrier()
        if stop == "P5":
            raise _Stop()
        wst = ExitStack()

        def res_w(name, src):
            t = wst.enter_context(nc.sbuf_tensor("rw_" + name, [128, 8, D], BF16))
            tk = Tok()
            K.dma("pool", t[:], src.rearrange("(kc p) c -> p kc c", p=128), K.slot("w_" + name), [], [tk])
            return t, tk
        Wa, Wat = res_w("a", wa_d)
        Wb, Wbt = res_w("b", wb_d)
        Wo_, Wot = res_w("out", wout_d)
        Wq, Wqt = res_w("q", wq_d)
        Wxo, Wxot = res_w("xo", wo_d)
        Wr = wst.enter_context(nc.sbuf_tensor("rw_r", [128, 8, 36], BF16))
        Wrt = Tok()
        K.dma("pool", Wr[:], wr_d.rearrange("(kc p) c -> p kc c", p=128), K.slot("w_r"), [], [Wrt])

        with ExitStack() as p6:
            names = ("qinT", "kinT", "kdecb", "ib", "ogB")
            srcs = {"qinT": qinT_s, "kinT": kinT_s, "kdecb": kdb_s, "ib": ib_s, "ogB": ogB_s}
            ld_r = {n: Rot(nc, p6, "hl_" + n, [128, 1024], BF16, 3) for n in names}
            iT_r = Rot(nc, p6, "h_iT", [128, 4, 64], BF16, 6)
            ob_r = Rot(nc, p6, "h_ob", [128, 8, 128], F32, 2)
            YR = {"sqj": Rot(nc, p6, "h_sqj", [128, 8, 128], F32, 1), "ss": Rot(nc, p6, "h_ss", [128, 8], F32, 2),
                  "yb": Rot(nc, p6, "h_yb", [128, 8, 128], BF16, 2), "yT": Rot(nc, p6, "h_yT", [128, 8, 128], BF16, 2)}
            S32 = p6.enter_context(nc.sbuf_tensor("h_S32", [128, 8, 128], F32))
            Sb = p6.enter_context(nc.sbuf_tensor("h_Sb", [128, 8, 128], BF16))
            s32t = [Tok(), Tok()]
            sbt = [Tok(), Tok()]
            for g in range(2):
                K.op("dve", lambda e: e.memset(S32[:, 4 * g:4 * g + 4, :], 0.0), [], [s32t[g]])
                K.op("dve", lambda e: e.memset(Sb[:, 4 * g:4 * g + 4, :], 0.0), [], [sbt[g]])
            H3 = lambda ap: ap.rearrange("p (h j) -> p h j", h=4)

            def load_block(b):
                bufs = {}
                bc = slice(b * 128, (b + 1) * 128)
                for n in names:
                    buf, _ = ld_r[n].get()
                    bufs[n] = buf
                i = ld_r["qinT"].i
                sl_ = K.slot("gld%d" % i)
                prev = ld_r["qinT"].toks[i]
                for n in ("qinT", "kinT"):
                    K.dma("sp", bufs[n][:].rearrange("p (h t) -> p h t", h=8), srcs[n][:, bc].rearrange("(h p) t -> p h t", p=128),
                          sl_, [DT[n]], [prev])
                for n in ("kdecb", "ib", "ogB"):
                    K.dma("sp", bufs[n][:], srcs[n][bc, :], sl_, [DT[n]], [prev])
                return bufs, prev

            nxt = load_block(0)
            for b in range(NT):
                L, ldt = nxt
                if b + 1 < NT:
                    nxt = load_block(b + 1)
                bc = slice(b * 128, (b + 1) * 128)
                qin = L["qinT"][:].rearrange("p (h t) -> p h t", h=8)
                kin = L["kinT"][:].rearrange("p (h t) -> p h t", h=8)
                kdb, ibb = L["kdecb"], L["ib"]
                o_blk, obt = ob_r.get()

                def hg(g):
                    def intra(ci):
                        r = slice(ci * 64, ci * 64 + 64)
                        cc = slice(ci * 64, ci * 64 + 64)
                        psI, tI = PS.get()
                        for hl in range(4):
                            h = 4 * g + hl
                            K.mm(psI[r, hl * 64:(hl + 1) * 64], kin[:, h, cc], qin[:, h, cc], True, True, [ldt], [tI])
                        iT, iTt = iT_r.get()
                        K.tt("dve", iT[r, 0:4, :], psI[r, 0:256].rearrange("p (h i) -> p h i", h=4),
                             C.maskT64[r, :].unsqueeze(1).to_broadcast([64, 4, 64]), ALU.mult, [tI, ctok], [iTt])
                        return iT, iTt
                    cur = intra(0)
                    yield
                    for ci in range(2):
                        r = slice(ci * 64, ci * 64 + 64)
                        cc = slice(ci * 64, ci * 64 + 64)
                        ch = 2 * b + ci
                        iT, iTt = cur
                        psO, tO = PS.get()
                        for hl in range(4):
                            h = 4 * g + hl
                            c = slice(hl * 128, (hl + 1) * 128)
                            K.mm(psO[r, c], qin[:, h, cc], Sb[:, h, :], True, False, [ldt, sbt[g]], [tO])
                            K.mm(psO[r, c], iT[r, hl, :], ibb[r, h * 128:(h + 1) * 128], False, True, [iTt, ldt], [tO])
                        K.cp("act", o_blk[r, 4 * g:4 * g + 4, :], H3(psO[r, :]), [tO], [obt])
                        psS, tS = PS.get()
                        for hl in range(4):
                            h = 4 * g + hl
                            c = slice(hl * 128, (hl + 1) * 128)
                            K.mm(psS[:, c], kdb[r, h * 128:(h + 1) * 128], ibb[r, h * 128:(h + 1) * 128], True, True, [ldt], [tS])
                        Sg = S32[:, 4 * g:4 * g + 4, :]
                        K.tt("dve", Sg, Sg, elast[:, 4 * g:4 * g + 4, ch:ch + 1].to_broadcast([128, 4, 128]), ALU.mult,
                             [s32t[g], elt], [s32t[g]])
                        K.tt("dve", Sg, Sg, H3(psS[:]), ALU.add, [s32t[g], tS], [s32t[g]])
                        K.cp("act", Sb[:, 4 * g:4 * g + 4, :], Sg, [s32t[g]], [sbt[g]])
                        if ci == 0:
                            cur = intra(1)
                        yield

                run_lockstep2([hg(0), hg(1)])
                if ob_s is not None:
                    K.dma("sp", ob_s[bc, :], o_blk[:].rearrange("p h d -> p (h d)"), K.slot("oa%d" % ob_r.i), [obt], [DT["o_b"]])
                finish_y("b", o_blk, obt, hwB, hwt, L["ogB"][:].rearrange("p (h d) -> p h d", h=8), ldt, ybT_s, "ybT", b, YR)
            K.barrier()
    if stop == "P6":
        raise _Stop()

    h2_s = scratch("h2", [S, D], F32)
    hn3T_s = scratch("hn3T", [D, S], BF16) if "hn3T" in dbg else None
    hn3k_s = scratch("hn3k", [S + 1, D], BF16)
    meta_s = scratch("meta", [NSLOT, 3], F32)
    ytok_s = scratch("ytok", [2 * S, D], BF16)
    for n in ("hn3k", "meta", "ytok"):
        DT[n] = Tok(nowaw=True)
    comb_s = scratch("comb", [S, NE], F32)
    h1_s = scratch("h1", [S, D], F32) if "h1" in dbg else None
    for n in ("h2", "hn3T", "comb", "h1"):
        DT[n] = Tok(nowaw=True)
    TT = 256
    with ExitStack() as ph:
        brB = ph.enter_context(nc.sbuf_tensor("brB", [128, 36], F32))
        brt = Tok()
        K.dma("sp", brB[:], br_d.partition_broadcast(128), K.slot("brB"), [], [brt])
        wBx = ph.enter_context(nc.sbuf_tensor("wBx", [128, D], F32))
        wBf = ph.enter_context(nc.sbuf_tensor("wBf", [128, D], F32))
        wBxt, wBft = Tok(), Tok()
        K.dma("sp", wBx[:], nw_d["nw_xa"].partition_broadcast(128), K.slot("wBx"), [], [wBxt])
        K.dma("sp", wBf[:], nw_d["nw_ffn"].partition_broadcast(128), K.slot("wBf"), [], [wBft])
        KT = ph.enter_context(nc.sbuf_tensor("x_KT", [128, 8, NMEM], BF16))
        V = ph.enter_context(nc.sbuf_tensor("x_V", [128, 2, D], BF16))
        KTt, Vt = Tok(), Tok()
        tmp = {"junk": Rot(nc, ph, "n_junk", [128, D], BF16, 1), "st": Rot(nc, ph, "n_st", [128, 4], F32, 4),
               "xs": Rot(nc, ph, "n_xs", [128, D], BF16, 2)}
        with ExitStack() as pm:
            wBm = pm.enter_context(nc.sbuf_tensor("wBm", [128, D], F32))
            wBmt = Tok()
            K.dma("sp", wBm[:], nw_d["nw_mem"].partition_broadcast(128), K.slot("wBm"), [], [wBmt])
            memnT = pm.enter_context(nc.sbuf_tensor("memnT", [128, 8, NMEM], BF16))
            mnt = Tok()
            mr = Rot(nc, pm, "m_x", [128, D], F32, 2)
            wkr = Rot(nc, pm, "m_w", [128, 8, 512], BF16, 2)
            for mt in range(2):
                xt, xtok = mr.get()
                K.dma("sp", xt[:], mem_d[mt * 128:(mt + 1) * 128, :], K.slot("xt%d" % mt), [], [xtok])
                norm_tile(xt[:], xtok, wBm[:], wBmt, memnT[:, :, mt * 128:(mt + 1) * 128], mnt, tmp)
            for blk in range(4):
                wk, wkt = wkr.get()
                K.dma("pool", wk[:], wkv_d[:, blk * 512:(blk + 1) * 512].rearrange("(kc p) c -> p kc c", p=128),
                      K.slot("wblk%d" % wkr.i), [], [wkt])
                if blk < 2:
                    for cc in range(4):
                        dc = blk * 4 + cc
                        ps, pst = PS.get()
                        for kc in range(8):
                            K.mm(ps[:, 0:NMEM], wk[:, kc, cc * 128:(cc + 1) * 128], memnT[:, kc, :], kc == 0, kc == 7, [wkt, mnt], [pst])
                        K.cp("act", KT[:, dc, :], ps[:, 0:NMEM], [pst], [KTt])
                else:
                    for mt in range(2):
                        ps, pst = PS.get()
                        for kc in range(8):
                            K.mm(ps[:], memnT[:, kc, mt * 128:(mt + 1) * 128], wk[:, kc, :], kc == 0, kc == 7, [wkt, mnt], [pst])
                        K.cp("act", V[:, mt, (blk - 2) * 512:(blk - 1) * 512], ps[:], [pst], [Vt])
            K.barrier()
        NS = TT // 128
        ldn = ("yaT", "ybT", "sgAT", "sgBT")
        lsrc = {"yaT": yaT_s, "ybT": ybT_s, "sgAT": sgAT_s, "sgBT": sgBT_s}
        ld_r = {n: Rot(nc, ph, "t_" + n, [128, 8, TT], BF16, 2) for n in ldn}
        xh_r = Rot(nc, ph, "t_xh", [128, NS, D], F32, 2)
        mg_r = Rot(nc, ph, "t_mg", [128, 8, TT], BF16, 2)
        hn2_r = Rot(nc, ph, "t_hn2", [128, 8, TT], BF16, 1)
        q2_r = Rot(nc, ph, "t_q2", [128, 8, TT], BF16, 1)
        o2_r = Rot(nc, ph, "t_o2", [128, 8, TT], BF16, 1)
        hn3_r = Rot(nc, ph, "t_hn3", [128, 8, TT], BF16, 1)
        t1_r = Rot(nc, ph, "t_t1", [128, TT], F32, 2)
        t2_r = Rot(nc, ph, "t_t2", [128, TT], F32, 2)
        p_r = Rot(nc, ph, "t_p", [128, 4, NMEM], F32, 1)
        pn_r = Rot(nc, ph, "t_pn", [128, 4, NMEM], BF16, 1)
        pT_r = Rot(nc, ph, "t_pT", [128, 8, 128], BF16, 1)
        sm_r = Rot(nc, ph, "t_sm", [128, 16], F32, 2)
        bc_reg = nc.gpsimd.to_reg(NSLOT - 1)
        mi = ph.enter_context(nc.sbuf_tensor("t_mi", [128, 128, 3], F32))
        mit = Tok()
        K.op("dve", lambda e: e.memset(mi[:, :, 0:1], float(S)), [], [mit])
        K.op("dve", lambda e: e.memset(mi[:, :, 1:2], 0.0), [], [mit])
        K.op("dve", lambda e: e.memset(mi[:, :, 2:3], float(2 * S)), [], [mit])
        meta_init = Tok(nowaw=True)
        K.dma("sp", meta_s.rearrange("(p j) c -> p j c", p=128), mi[:], K.slot("minit"), [mit], [DT["meta"], meta_init])

        def load_tile(T):
            tc_ = slice(T * TT, (T + 1) * TT)
            bufs = {}
            for n in ldn:
                buf, _ = ld_r[n].get()
                bufs[n] = buf
            i = ld_r["yaT"].i
            prev = ld_r["yaT"].toks[i]
            for n in ldn:
                K.dma("sp", bufs[n][:], lsrc[n][:, tc_].rearrange("(k p) t -> p k t", p=128), K.slot("tld%d" % i), [DT[n]], [prev])
            xh, xht = xh_r.get()
            K.dma("sp", xh[:], x_d[tc_, :].rearrange("(s p) d -> p s d", p=128), K.slot("txh%d" % xh_r.i), [], [xht])
            return bufs, prev, xh, xht

        tiles = {}
        mgs = {}

        def genA(T):
            L, ldt, xh, xht = tiles[T]
            tc_ = slice(T * TT, (T + 1) * TT)
            mg, mgt = mg_r.get()
            for dc in range(8):
                c = slice(dc * 128, (dc + 1) * 128)
                psA, tA = PS.get()
                psB, tB = PS.get()
                for kc in range(8):
                    K.mm(psA[:, 0:TT], Wa[:, kc, c], L["yaT"][:, kc, :], kc == 0, kc == 7, [Wat, ldt], [tA])
                for kc in range(8):
                    K.mm(psB[:, 0:TT], Wb[:, kc, c], L["ybT"][:, kc, :], kc == 0, kc == 7, [Wbt, ldt], [tB])
                t1, t1t = t1_r.get()
                t2, t2t = t2_r.get()
                K.tt("dve", t1[:], psA[:, 0:TT], L["sgAT"][:, dc, :], ALU.mult, [tA, ldt], [t1t])
                K.tt("dve", t2[:], psB[:, 0:TT], L["sgBT"][:, dc, :], ALU.mult, [tB, ldt], [t2t])
                K.tt("pool", mg[:, dc, :], t1[:], t2[:], ALU.add, [t1t, t2t], [mgt])
                if dc % 2 == 1:
                    yield
            for sub in range(NS):
                for half in range(2):
                    ps, pst = PS.get()
                    for kc in range(8):
                        K.mm(ps[:], mg[:, kc, sub * 128:(sub + 1) * 128], Wo_[:, kc, half * 512:(half + 1) * 512], kc == 0, kc == 7,
                             [mgt, Wot], [pst])
                    K.tt("dve", xh[:, sub, half * 512:(half + 1) * 512], ps[:], xh[:, sub, half * 512:(half + 1) * 512], ALU.add,
                         [pst, xht], [xht])
            if h1_s is not None:
                K.dma("sp", h1_s[tc_, :].rearrange("(s p) d -> p s d", p=128), xh[:], K.slot("h1d"), [xht], [DT["h1"]])
            mgs[T] = (mg, mgt)
            yield

        def genB(T):
            L, ldt, xh, xht = tiles[T]
            tc_ = slice(T * TT, (T + 1) * TT)
            hn2, hn2t = hn2_r.get()
            for sub in range(NS):
                norm_tile(xh[:, sub, :], xht, wBx[:], wBxt, hn2[:, :, sub * 128:(sub + 1) * 128], hn2t, tmp)
            q2, q2t = q2_r.get()
            for dc in range(8):
                ps, pst = PS.get()
                for kc in range(8):
                    K.mm(ps[:, 0:TT], Wq[:, kc, dc * 128:(dc + 1) * 128], hn2[:, kc, :], kc == 0, kc == 7, [Wqt, hn2t], [pst])
                K.cp("act", q2[:, dc, :], ps[:, 0:TT], [pst], [q2t])
                if dc % 4 == 3:
                    yield
            o2, o2t = o2_r.get()
            for sub in range(NS):
                sc_ = slice(sub * 128, (sub + 1) * 128)
                pss = [PS.get(), PS.get()]
                for hd in range(4):
                    psx, tx = pss[hd // 2]
                    for j in range(2):
                        K.mm(psx[:, (hd % 2) * 256:(hd % 2) * 256 + 256], q2[:, 2 * hd + j, sc_], KT[:, 2 * hd + j, :], j == 0, j == 1,
                             [q2t, KTt], [tx])
                sm, smt = sm_r.get()
                for i2 in range(2):
                    psx, tx = pss[i2]
                    K.op("dve", lambda e: e.tensor_reduce(out=sm[:, 2 * i2:2 * i2 + 2], in_=psx[:].rearrange("p (h m) -> p h m", h=2),
                                                          axis=AX.X, op=ALU.max), [tx], [smt])
                K.ts("dve", sm[:, 4:8], sm[:, 0:4], -1.0 / 16.0, None, ALU.mult, None, [smt], [smt])
                p, pt = p_r.get()
                for hd in range(4):
                    psx, tx = pss[hd // 2]
                    K.act(p[:, hd, :], psx[:, (hd % 2) * 256:(hd % 2) * 256 + 256], AF.Exp, [tx, smt], [pt, smt],
                          scale=1.0 / 16.0, bias=sm[:, 4 + hd:5 + hd], accum_out=sm[:, 8 + hd:9 + hd])
                K.op("dve", lambda e: e.reciprocal(out=sm[:, 12:16], in_=sm[:, 8:12]), [smt], [smt])
                pn, pnt = pn_r.get()
                K.tt("pool", pn[:], p[:], sm[:, 12:16].unsqueeze(2).to_broadcast([128, 4, NMEM]), ALU.mult, [pt, smt], [pnt])
                yield
                ps, pst = PS.get()
                psb = ps[:].bitcast(BF16)
                for hd in range(4):
                    for mc in range(2):
                        K.tr(psb[:, (hd * 2 + mc) * 128:(hd * 2 + mc + 1) * 128], pn[:, hd, mc * 128:(mc + 1) * 128], C.identb[:],
                             [pnt, ctok], [pst])
                pT, pTt = pT_r.get()
                K.cp("act", pT[:], psb.rearrange("p (a t) -> p a t", a=8), [pst], [pTt])
                yield
                for og in range(2):
                    pso, tso = PS.get()
                    for d4 in range(4):
                        dc = og * 4 + d4
                        hd = dc // 2
                        for mc in range(2):
                            K.mm(pso[:, d4 * 128:(d4 + 1) * 128], V[:, mc, dc * 128:(dc + 1) * 128], pT[:, hd * 2 + mc, :], mc == 0, mc == 1,
                                 [Vt, pTt], [tso])
                    K.cp("act", o2[:, og * 4:og * 4 + 4, sc_], pso[:].rearrange("p (a t) -> p a t", a=4), [tso], [o2t])
                    yield
            for sub in range(NS):
                for half in range(2):
                    ps, pst = PS.get()
                    for kc in range(8):
                        K.mm(ps[:], o2[:, kc, sub * 128:(sub + 1) * 128], Wxo[:, kc, half * 512:(half + 1) * 512], kc == 0, kc == 7,
                             [o2t, Wxot], [pst])
                    K.tt("dve", xh[:, sub, half * 512:(half + 1) * 512], ps[:], xh[:, sub, half * 512:(half + 1) * 512], ALU.add,
                         [pst, xht], [xht])
            K.dma("sp", h2_s[tc_, :].rearrange("(s p) d -> p s d", p=128), xh[:], K.slot("h2d%d" % xh_r.i), [xht], [DT["h2"]])
            yield
            hn3, hn3t = hn3_r.get()
            for sub in range(NS):
                xs_, xst_ = norm_tile(xh[:, sub, :], xht, wBf[:], wBft, hn3[:, :, sub * 128:(sub + 1) * 128], hn3t, tmp)
                r0 = T * TT + sub * 128
                K.dma("sp", hn3k_s[r0:r0 + 128, :], xs_[:], K.slot("hn3k%d" % tmp["xs"].i), [xst_], [DT["hn3k"]])
            if hn3T_s is not None:
                K.dma("sp", hn3T_s[:, tc_].rearrange("(k p) t -> p k t", p=128), hn3[:], K.slot("hn3d%d" % hn3_r.i), [hn3t], [DT["hn3T"]])
            for sub in range(NS):
                ps, pst = PS.get()
                for kc in range(8):
                    K.mm(ps[:, 0:36], hn3[:, kc, sub * 128:(sub + 1) * 128], Wr[:, kc, :], kc == 0, kc == 7, [hn3t, Wrt], [pst])
                si_ = T * NS + sub
                K.tt("dve", lgall[:, si_, :], ps[:, 0:36], brB[:], ALU.add, [pst, brt], [lgt])
            yield

        NTL = S // TT
        tiles[0] = load_tile(0)
        if NTL > 1:
            tiles[1] = load_tile(1)
        for _ in genA(0):
            pass
        for T in range(NTL):
            gens = [genB(T)]
            if T + 1 < NTL:
                gens.append(genA(T + 1))
            run_lockstep2(gens)
            del tiles[T]
            if T + 2 < NTL:
                tiles[T + 2] = load_tile(T + 2)
        K.barrier()
    wst.close()
    with ExitStack() as pr:
        A3 = lambda name, dt_=F32: pr.enter_context(nc.sbuf_tensor(name, [128, NT, NE], dt_))
        A2 = lambda name, dt_=F32: pr.enter_context(nc.sbuf_tensor(name, [128, NT], dt_))
        el, ex, m_, cw, rk, cs, pa, pb = (A3("r_%s" % n) for n in ("el", "ex", "m", "cw", "rk", "cs", "pa", "pb"))
        mb = A3("r_mb", BF16)
        g4 = pr.enter_context(nc.sbuf_tensor("r_g4", [128, NT, 4], F32))
        oh = pr.enter_context(nc.sbuf_tensor("r_oh", [128, NT, 4], F32))
        gmx, gs, l1, l2, den, k1, k2, w1, w2, t2a = (A2("r2_%s" % n) for n in ("gmx", "gs", "l1", "l2", "den", "k1", "k2", "w1", "w2", "t2a"))
        scin = pr.enter_context(nc.sbuf_tensor("r_scin", [128, NT, 2, 3], F32))
        posf = pr.enter_context(nc.sbuf_tensor("r_posf", [128, NT, 2], F32))
        posi = pr.enter_context(nc.sbuf_tensor("r_posi", [128, NT, 2], I32))
        rt = Tok()
        B3 = lambda a2: a2[:].unsqueeze(2).to_broadcast([128, NT, NE])
        Gv = lgall[:, :, 0:4]
        Ev = lgall[:, :, 4:36]
        RD = lambda out, in_, op: K.op("dve", lambda e: e.tensor_reduce(out=out, in_=in_, axis=AX.X, op=op), [rt, lgt], [rt])
        TTd = lambda out, a, b, op: K.tt("dve", out, a, b, op, [rt, lgt, ctok], [rt])
        RD(gmx[:], Gv, ALU.max)
        TTd(g4[:], Gv, gmx[:].unsqueeze(2).to_broadcast([128, NT, 4]), ALU.subtract)
        TTd(oh[:], Gv, gmx[:].unsqueeze(2).to_broadcast([128, NT, 4]), ALU.is_ge)
        K.act(g4[:], g4[:], AF.Exp, [rt], [rt])
        RD(gs[:], g4[:], ALU.add)
        K.op("dve", lambda e: e.reciprocal(out=gs[:], in_=gs[:]), [rt], [rt])
        K.ts("dve", oh[:], oh[:], -1.0, 1.0e9, ALU.add, ALU.mult, [rt], [rt])
        TTd(el[:].rearrange("p s (g e) -> p s g e", g=4), Ev.rearrange("p s (g e) -> p s g e", g=4),
            oh[:].unsqueeze(3).to_broadcast([128, NT, 4, 8]), ALU.add)
        RD(l1[:], el[:], ALU.max)
        TTd(m_[:], el[:], B3(l1), ALU.is_ge)
        K.stt("dve", ex[:], m_[:], -1.0e9, el[:], ALU.mult, ALU.add, [rt], [rt])
        RD(l2[:], ex[:], ALU.max)
        TTd(ex[:], el[:], B3(l1), ALU.subtract)
        K.act(ex[:], ex[:], AF.Exp, [rt], [rt])
        TTd(m_[:], el[:], B3(l2), ALU.is_ge)
        TTd(cw[:], ex[:], m_[:], ALU.mult)
        RD(den[:], cw[:], ALU.add)
        K.op("dve", lambda e: e.reciprocal(out=den[:], in_=den[:]), [rt], [rt])
        TTd(den[:], den[:], gs[:], ALU.mult)
        TTd(cw[:], cw[:], B3(den), ALU.mult)
        if "comb" in dbg:
            K.dma("sp", comb_s.rearrange("(s p) e -> p s e", p=128), cw[:], K.slot("cbd"), [rt], [DT["comb"]])
        K.ts("dve", mb[:], cw[:], 0.0, None, ALU.is_gt, None, [rt], [rt])
        for half in range(2):
            psR, tR = PS.get()
            psC, tC = PS.get()
            for j in range(16):
                s_ = half * 16 + j
                K.mm(psR[:, j * NE:(j + 1) * NE], C.UT[:], mb[:, s_, :], True, True, [rt, ctok], [tR])
                K.mm(psC[:, j * NE:(j + 1) * NE], C.onesb[:], mb[:, s_, :], True, True, [rt, ctok], [tC])
            K.cp("dve", rk[:, half * 16:(half + 1) * 16, :], psR[:].rearrange("p (s e) -> p s e", e=NE), [tR], [rt])
            K.cp("dve", cs[:, half * 16:(half + 1) * 16, :], psC[:].rearrange("p (s e) -> p s e", e=NE), [tC], [rt])
        src = cs
        for k_, dst in zip((1, 2, 4, 8, 16), (pa, pb, pa, pb, pa)):
            K.cp("dve", dst[:, 0:k_, :], src[:, 0:k_, :], [rt], [rt])
            TTd(dst[:, k_:NT, :], src[:, k_:NT, :], src[:, 0:NT - k_, :], ALU.add)
            src = dst
        TTd(pb[:], src[:], cs[:], ALU.subtract)
        TTd(rk[:], rk[:], pb[:], ALU.add)
        TTd(pa[:], rk[:], C.ecap[:].unsqueeze(1).to_broadcast([128, NT, NE]), ALU.add)
        K.ts("dve", pb[:], rk[:], float(CAP), None, ALU.is_lt, None, [rt], [rt])
        TTd(pb[:], pb[:], mb[:], ALU.mult)
        K.ts("dve", pa[:], pa[:], -1.0, BIG, ALU.mult, ALU.add, [rt], [rt])
        TTd(pa[:], pa[:], pb[:], ALU.mult)
        RD(k1[:], pa[:], ALU.max)
        TTd(m_[:], pa[:], B3(k1), ALU.is_ge)
        K.stt("dve", ex[:], m_[:], -2.0 * BIG, pa[:], ALU.mult, ALU.add, [rt], [rt])
        RD(k2[:], ex[:], ALU.max)
        K.ts("dve", k2[:], k2[:], 0.0, None, ALU.max, None, [rt], [rt])
        for kk, wk, col in ((k1, w1, 0), (k2, w2, 1)):
            TTd(m_[:], pa[:], B3(kk), ALU.is_equal)
            TTd(m_[:], m_[:], cw[:], ALU.mult)
            RD(wk[:], m_[:], ALU.add)
            K.cp("dve", scin[:, :, col, 1], wk[:], [rt], [rt])
            K.cp("dve", scin[:, :, col, 0], C.tokall[:], [rt, ctok], [rt])
            K.ts("dve", scin[:, :, col, 2], C.tokall[:], float(col * S), None, ALU.add, None, [rt, ctok], [rt])
            K.ts("dve", posf[:, :, col], kk[:], -1.0, BIG, ALU.mult, ALU.add, [rt], [rt])
        K.cp("dve", posi[:], posf[:], [rt], [rt])
        for s_ in range(NT):
            for k2_ in range(2):
                K.idma(meta_s[:, :], bass.IndirectOffsetOnAxis(ap=posi[:, s_, k2_:k2_ + 1], axis=0), scin[:, s_, k2_, :], None,
                       K.slot("msc%d" % ((2 * s_ + k2_) % 8)), [rt, meta_init], [DT["meta"]],
                       bounds_check=bc_reg, oob_is_err=False)
        K.barrier()
    if stop == "P8":
        raise _Stop()

    with ExitStack() as ph:
        wBn = ph.enter_context(nc.sbuf_tensor("wBn", [128, D], F32))
        wBnt = Tok()
        K.dma("sp", wBn[:], nw_d["nw_fin"].partition_broadcast(128), K.slot("wB"), [], [wBnt])
        NTE = CAP // 128
        with ExitStack() as pe_:
            wg_r = Rot(nc, pe_, "e_wg", [128, 8, DFF], BF16, 4)
            wu_r = Rot(nc, pe_, "e_wu", [128, 8, DFF], BF16, 4)
            wd_r = Rot(nc, pe_, "e_wd", [128, 4, D], BF16, 4)
            mt_r = Rot(nc, pe_, "e_mt", [128, NTE, 3], F32, 4)
            ix_r = Rot(nc, pe_, "e_ix", [128, 1], I32, 4 * NTE)
            xg_r = Rot(nc, pe_, "e_xg", [128, D], BF16, 12)
            XT_r = Rot(nc, pe_, "e_XT", [128, 8, CAP], BF16, 3)
            hT_r = Rot(nc, pe_, "e_hT", [128, 4, CAP], BF16, 3)
            sg_r = Rot(nc, pe_, "e_sg", [128, CAP], F32, 2)
            ys_r = Rot(nc, pe_, "e_ys", [128, D], BF16, 4)
            zr2 = pe_.enter_context(nc.sbuf_tensor("e_zr", [128, D], BF16))
            zr2t = Tok()
            ytok_init = Tok(nowaw=True)
            K.op("dve", lambda e: e.memset(zr2[:], 0.0), [], [zr2t])
            K.dma("sp", hn3k_s[S:S + 1, :], zr2[0:1, :], K.slot("zini"), [zr2t], [DT["hn3k"]])
            for j_ in range(2 * S // 1024):
                K.dma("sp", ytok_s[j_ * 1024:(j_ + 1) * 1024, :].rearrange("(a p) d -> p a d", p=128),
                      zr2[:].unsqueeze(1).to_broadcast([128, 8, D]), K.slot("zini"), [zr2t], [DT["ytok"], ytok_init])
            dx_r = Rot(nc, pe_, "e_dx", [128, 1], I32, 8)
            bc2_reg = nc.gpsimd.to_reg(2 * S - 1)
            def expert_gen(e_):
                wg, wgt = wg_r.get()
                wu, wut = wu_r.get()
                wd, wdt = wd_r.get()
                i = wg_r.i
                K.dma("pool", wg[:], wg_d[e_].rearrange("(kc p) f -> p kc f", p=128), K.slot("ewg%d" % i), [], [wgt])
                K.dma("pool", wu[:], wu_d[e_].rearrange("(kc p) f -> p kc f", p=128), K.slot("ewu%d" % i), [], [wut])
                K.dma("pool", wd[:], wd_d[e_].rearrange("(fc p) d -> p fc d", p=128), K.slot("ewd%d" % i), [], [wdt])
                mt, mtt = mt_r.get()
                K.dma("sp", mt[:], meta_s[e_ * CAP:(e_ + 1) * CAP, :].rearrange("(t p) c -> p t c", p=128), K.slot("emt%d" % mt_r.i),
                      [DT["meta"]], [mtt])
                xgs = []
                for t in range(NTE):
                    ix, ixt = ix_r.get()
                    K.cp("dve", ix[:], mt[:, t, 0:1], [mtt], [ixt])
                    xg, xgt = xg_r.get()
                    K.idma(xg[:, :], None, hn3k_s[:, :], bass.IndirectOffsetOnAxis(ap=ix[:, :], axis=0),
                           K.slot("exg%d" % xg_r.i), [ixt, DT["hn3k"]], [xgt])
                    xgs.append((xg, xgt))
                yield
                XT, XTt = XT_r.get()
                for t in range(NTE):
                    xg, xgt = xgs[t]
                    ps, pst = PS.get()
                    psb = ps[:].bitcast(BF16)
                    for kc in range(8):
                        K.tr(psb[:, kc * 128:(kc + 1) * 128], xg[:, kc * 128:(kc + 1) * 128], C.identb[:], [xgt, ctok], [pst])
                    K.cp("act", XT[:, :, t * 128:(t + 1) * 128], psb.rearrange("p (k t) -> p k t", k=8), [pst], [XTt])
                yield
                hT, hTt = hT_r.get()
                for fc in range(4):
                    c = slice(fc * 128, (fc + 1) * 128)
                    psG, tG = PS.get()
                    psU, tU = PS.get()
                    for kc in range(8):
                        K.mm(psG[:, 0:CAP], wg[:, kc, c], XT[:, kc, :], kc == 0, kc == 7, [wgt, XTt], [tG])
                    for kc in range(8):
                        K.mm(psU[:, 0:CAP], wu[:, kc, c], XT[:, kc, :], kc == 0, kc == 7, [wut, XTt], [tU])
                    sg, sgt = sg_r.get()
                    K.act(sg[:], psG[:, 0:CAP], AF.Silu, [tG], [sgt])
                    K.tt("dve", hT[:, fc, :], sg[:], psU[:, 0:CAP], ALU.mult, [sgt, tU], [hTt])
                yield
                for t in range(NTE):
                    ys, yst = ys_r.get()
                    for half in range(2):
                        psY, tY = PS.get()
                        for fc in range(4):
                            K.mm(psY[:], hT[:, fc, t * 128:(t + 1) * 128], wd[:, fc, half * 512:(half + 1) * 512], fc == 0, fc == 3,
                                 [hTt, wdt], [tY])
                        if half == 0:
                            K.act(ys[:, 0:512], psY[:], AF.Copy, [tY, mtt], [yst], scale=mt[:, t, 1:2])
                        else:
                            K.ts("dve", ys[:, 512:1024], psY[:], mt[:, t, 1:2], None, ALU.mult, None, [tY, mtt], [yst])
                    dx, dxt = dx_r.get()
                    K.cp("dve", dx[:], mt[:, t, 2:3], [mtt], [dxt])
                    K.idma(ytok_s[:, :], bass.IndirectOffsetOnAxis(ap=dx[:, :], axis=0), ys[:, :], None,
                           K.slot("eys%d" % ys_r.i), [yst, dxt, ytok_init], [DT["ytok"]],
                           bounds_check=bc2_reg, oob_is_err=False)
                yield

            run_skewed((expert_gen(e_) for e_ in range(NE)), 4)
            K.barrier()
        acc_r = Rot(nc, ph, "f_acc", [128, D], F32, 8)
        y1_r = Rot(nc, ph, "f_y1", [128, D], BF16, 8)
        y2_r = Rot(nc, ph, "f_y2", [128, D], BF16, 8)
        st_r = Rot(nc, ph, "f_st", [128, 4], F32, 4)
        jk_r = Rot(nc, ph, "f_jk", [128, D], BF16, 2)
        ot_r = Rot(nc, ph, "f_ot", [128, D], F32, 4)
        def fload(t):
            r0 = t * 128
            acc, acct = acc_r.get()
            i = acc_r.i
            K.dma("sp", acc[:], h2_s[r0:r0 + 128, :], K.slot("facc%d" % i), [DT["h2"]], [acct])
            y1, y1t = y1_r.get()
            y2, y2t = y2_r.get()
            K.dma("sp", y1[:], ytok_s[r0:r0 + 128, :], K.slot("fy1%d" % i), [DT["ytok"]], [y1t])
            K.dma("sp", y2[:], ytok_s[S + r0:S + r0 + 128, :], K.slot("fy2%d" % i), [DT["ytok"]], [y2t])
            return acc, acct, y1, y1t, y2, y2t

        LA = 7
        pend = [fload(t) for t in range(min(LA, NT))]
        for t in range(NT):
            r0 = t * 128
            acc, acct, y1, y1t, y2, y2t = pend.pop(0)
            K.tt("dve", acc[:], acc[:], y1[:], ALU.add, [acct, y1t], [acct])
            K.tt("pool", acc[:], acc[:], y2[:], ALU.add, [acct, y2t], [acct])
            jk, jkt = jk_r.get()
            st, stt_ = st_r.get()
            K.act(jk[:], acc[:], AF.Square, [acct], [jkt, stt_], accum_out=st[:, 0:1])
            K.act(st[:, 1:2], st[:, 0:1], AF.Sqrt, [stt_, ctok], [stt_], scale=1.0 / D, bias=C.epsc[:, 0:1])
            K.op("dve", lambda e: e.reciprocal(out=st[:, 2:3], in_=st[:, 1:2]), [stt_], [stt_])
            ot, ott = ot_r.get()
            K.stt("dve", ot[:], acc[:], st[:, 2:3], wBn[:], ALU.mult, ALU.mult, [acct, stt_, wBnt], [ott])
            K.dma("sp", out_d[r0:r0 + 128, :], ot[:], K.slot("eout%d" % ot_r.i), [ott], [])
            if t + LA < NT:
                pend.append(fload(t + LA))
        K.barrier()


def make_in_maps(inputs):
    hc = host_consts()
    f = lambda a: np.ascontiguousarray(np.asarray(a, dtype=np.float32))
    shared = {
        "w_in": f(inputs["w_in"][0]),
        "cw": f(np.asarray(inputs["conv_w"][0]).T.reshape(24, 128, 4).transpose(1, 0, 2)),
        "a_log": f(inputs["gdn_a_log"][0]).reshape(8, 1),
        "dt_bias": f(inputs["gdn_dt_bias"][0]).reshape(8, 1),
        "gdn_nw": f(inputs["gdn_out_norm_w"][0]).reshape(1, 128),
        "hgrn_nw": f(inputs["hgrn_out_norm_w"][0]).reshape(1, 128),
        "lbT": f(np.asarray(inputs["hgrn_lb"]).reshape(2, 8, 128).transpose(2, 0, 1)),
        "w_a": f(inputs["w_branch_a"][0]), "w_b": f(inputs["w_branch_b"][0]), "w_out": f(inputs["w_out"][0]),
        "wq": f(inputs["xattn_wq"][0]), "wkv": f(inputs["xattn_wkv"][0]), "wo": f(inputs["xattn_wo"][0]),
        "nw_mix": f(inputs["norm_mix_w"][0]).reshape(1, D), "nw_xa": f(inputs["norm_xattn_w"][0]).reshape(1, D),
        "nw_mem": f(inputs["norm_mem_w"][0]).reshape(1, D), "nw_ffn": f(inputs["norm_ffn_w"][0]).reshape(1, D),
        "nw_fin": f(inputs["final_norm_w"]).reshape(1, D),
        "wr": f(np.concatenate([np.asarray(inputs["router_group_w"][0]), np.asarray(inputs["router_expert_w"][0])], axis=1)),
        "br": f(np.concatenate([np.asarray(inputs["router_group_b"][0]), np.asarray(inputs["router_expert_b"][0])])).reshape(1, 36),
        "wg": f(inputs["expert_w_gate"][0]), "wu": f(inputs["expert_w_up"][0]), "wd": f(inputs["expert_w_down"][0]),
    }
    for k, v in hc.items():
        shared["c_" + k] = np.ascontiguousarray(v)
    shared["c_rmask"] = host_rmask()
    maps = []
    for b in range(8):
        m = dict(shared)
        m["x"] = f(inputs["x"][b])
        m["mem"] = f(inputs["mem"][b])
        maps.append(m)
    return maps


def kernel(**inputs):
    nc = build()
    in_maps = make_in_maps(inputs)
    res = run_bass_kernel_spmd(nc, in_maps, core_ids=list(range(8)))
    return np.stack([np.asarray(r["out"], dtype=np.float32) for r in res.results], axis=0)
```
